# Optimizing a Trainium2 kernel written in Bass

```python
import math
import jax
import jax.numpy as jnp
from jax import lax
import numpy as np

D_MODEL = 1024
BATCH = 8
SEQ = 4096
DEPTH = 2

GLA_HEADS = 4
GLA_DK = D_MODEL // 16
GLA_DV = D_MODEL // 8
GLA_RANK = 16
GLA_GATE_NORM = 16.0
ML_HEADS = 4
ML_DK = D_MODEL // 16
ML_DV = D_MODEL // 8
ML_CONV = 4
S5_GROUP = 16
S5_WIDTH = D_MODEL // 2
S5_GROUPS = S5_WIDTH // S5_GROUP
S5_STATE = 64
S5_DT_MIN = 0.001
S5_DT_MAX = 0.1
CHUNK = 64
FFN_HIDDEN = ((8 * D_MODEL + 767) // 768) * 256
N_BRANCH = 3
RMS_EPS = 1e-6

IN_SIZES = (
    GLA_HEADS * GLA_DK,
    GLA_HEADS * GLA_DK,
    GLA_HEADS * GLA_DV,
    GLA_RANK,
    GLA_HEADS * GLA_DV,
    ML_HEADS * ML_DK,
    ML_HEADS * ML_DK,
    ML_HEADS * ML_DV,
    ML_HEADS,
    ML_HEADS,
    ML_HEADS * ML_DV,
    S5_WIDTH,
    N_BRANCH * D_MODEL,
)
IN_DIM = sum(IN_SIZES)

kernel_name = 'hybrid_gla_mlstm_s5_block'


def rms_norm(x, w):
    xf = x.astype(jnp.float32)
    y = xf * lax.rsqrt(jnp.mean(xf * xf, axis=-1, keepdims=True) + RMS_EPS)
    return (y * w.astype(jnp.float32)).astype(x.dtype)


def _split(z, sizes):
    out, start = [], 0
    for s in sizes:
        out.append(z[..., start:start + s])
        start += s
    return out


def _heads(a, n):
    return a.reshape(a.shape[0], a.shape[1], n, -1)


def _to_chunks(a):
    b, t, h, d = a.shape
    return a.reshape(b, t // CHUNK, CHUNK, h, d).transpose(1, 0, 3, 2, 4)


def _from_chunks(a):
    n, b, h, l, d = a.shape
    return a.transpose(1, 0, 3, 2, 4).reshape(b, n * l, h, d)


def gla_attention(q, k, v, g):
    out_dtype = v.dtype
    f32 = jnp.float32
    bsz, _, heads, dk = q.shape
    dv = v.shape[-1]
    qc, kc, vc, gc = (_to_chunks(a.astype(f32)) for a in (q * dk ** -0.5, k, v, g))
    causal = jnp.tril(jnp.ones((CHUNK, CHUNK), dtype=bool))

    def step(state, inp):
        qi, ki, vi, gi = inp
        cum = jnp.cumsum(gi, axis=2)
        o_inter = jnp.einsum('bhld,bhdv->bhlv', qi * jnp.exp(cum), state)
        rel = cum[:, :, :, None, :] - cum[:, :, None, :, :]
        decay = jnp.exp(jnp.where(causal[:, :, None], rel, -jnp.inf))
        scores = jnp.einsum('bhid,bhjd,bhijd->bhij', qi, ki, decay)
        o = o_inter + jnp.einsum('bhij,bhjv->bhiv', scores, vi)
        last = cum[:, :, -1:, :]
        state = (jnp.exp(last[:, :, 0, :])[..., None] * state
                 + jnp.einsum('bhjd,bhjv->bhdv', ki * jnp.exp(last - cum), vi))
        return state, o

    s0 = jnp.zeros((bsz, heads, dk, dv), f32)
    _, o = lax.scan(step, s0, (qc, kc, vc, gc))
    return _from_chunks(o).astype(out_dtype)


def mlstm_memory(q, k, v, i_pre, f_pre):
    out_dtype = v.dtype
    f32 = jnp.float32
    bsz, _, heads, dk = q.shape
    dv = v.shape[-1]
    log_f = jax.nn.log_sigmoid(f_pre.astype(f32))
    qc, kc, vc = (_to_chunks(a.astype(f32)) for a in (q, k * dk ** -0.5, v))
    ic, fc = (_to_chunks(a.astype(f32)[..., None])[..., 0] for a in (i_pre, log_f))
    causal = jnp.tril(jnp.ones((CHUNK, CHUNK), dtype=bool))

    def step(carry, inp):
        mem, norm, m_prev = carry
        qi, ki, vi, ii, lfi = inp
        b = jnp.cumsum(lfi, axis=-1)
        a_inter = b + m_prev[..., None]
        d_intra = jnp.where(causal, b[..., :, None] - b[..., None, :] + ii[..., None, :], -jnp.inf)
        m_t = jnp.maximum(a_inter, jnp.max(d_intra, axis=-1))
        w_inter = jnp.exp(a_inter - m_t)
        scores = jnp.einsum('bhtd,bhsd->bhts', qi, ki) * jnp.exp(d_intra - m_t[..., None])
        num = (w_inter[..., None] * jnp.einsum('bhtd,bhdv->bhtv', qi, mem)
               + jnp.einsum('bhts,bhsv->bhtv', scores, vi))
        den = w_inter * jnp.einsum('bhtd,bhd->bht', qi, norm) + jnp.sum(scores, axis=-1)
        h = num / jnp.maximum(jnp.abs(den), jnp.exp(-m_t))[..., None]
        m_new = m_t[..., -1]
        carry_decay = jnp.exp(b[..., -1] + m_prev - m_new)
        w_key = jnp.exp(b[..., -1:] - b + ii - m_new[..., None])
        mem = carry_decay[..., None, None] * mem + jnp.einsum('bhs,bhsd,bhsv->bhdv', w_key, ki, vi)
        norm = carry_decay[..., None] * norm + jnp.einsum('bhs,bhsd->bhd', w_key, ki)
        return (mem, norm, m_new), h

    carry0 = (jnp.zeros((bsz, heads, dk, dv), f32),
              jnp.zeros((bsz, heads, dk), f32),
              jnp.zeros((bsz, heads), f32))
    _, h = lax.scan(step, carry0, (qc, kc, vc, ic, fc))
    return _from_chunks(h).astype(out_dtype)


def _complex_linear_combine(earlier, later):
    a1r, a1i, b1r, b1i = earlier
    a2r, a2i, b2r, b2i = later
    return (a2r * a1r - a2i * a1i,
            a2r * a1i + a2i * a1r,
            a2r * b1r - a2i * b1i + b2r,
            a2r * b1i + a2i * b1r + b2i)


def s5_ssm(u, lam_re, lam_im, log_step, b_re, b_im, c_re, c_im, d_skip):
    out_dtype = u.dtype
    f32 = jnp.float32
    bsz, t, _ = u.shape
    uf = u.astype(f32).reshape(bsz, t, S5_GROUPS, S5_GROUP)
    lam_re = jnp.minimum(lam_re.astype(f32), -1e-4)
    lam_im = lam_im.astype(f32)
    dt = jnp.exp(log_step.astype(f32))[:, None]
    mag = jnp.exp(lam_re * dt)
    abar_re = mag * jnp.cos(lam_im * dt)
    abar_im = mag * jnp.sin(lam_im * dt)
    inv = 1.0 / (lam_re * lam_re + lam_im * lam_im)
    z_re = ((abar_re - 1.0) * lam_re + abar_im * lam_im) * inv
    z_im = (abar_im * lam_re - (abar_re - 1.0) * lam_im) * inv
    b_re = b_re.astype(f32)
    b_im = b_im.astype(f32)
    bbar_re = z_re[..., None] * b_re - z_im[..., None] * b_im
    bbar_im = z_re[..., None] * b_im + z_im[..., None] * b_re
    bu_re = jnp.einsum('gpc,btgc->btgp', bbar_re, uf)
    bu_im = jnp.einsum('gpc,btgc->btgp', bbar_im, uf)
    a_re = jnp.broadcast_to(abar_re, (1, t) + abar_re.shape)
    a_im = jnp.broadcast_to(abar_im, (1, t) + abar_im.shape)
    _, _, x_re, x_im = lax.associative_scan(_complex_linear_combine, (a_re, a_im, bu_re, bu_im), axis=1)
    y = (jnp.einsum('gcp,btgp->btgc', c_re.astype(f32), x_re)
         - jnp.einsum('gcp,btgp->btgc', c_im.astype(f32), x_im))
    y = y.reshape(bsz, t, S5_WIDTH) + d_skip.astype(f32) * u.astype(f32)
    return y.astype(out_dtype)


def causal_dwconv(u, w, b):
    k = w.shape[0]
    y = lax.conv_general_dilated(u, w[:, None, :], window_strides=(1,), padding=[(k - 1, 0)],
                                 dimension_numbers=('NWC', 'WIO', 'NWC'),
                                 feature_group_count=u.shape[-1])
    return y + b


def hybrid_mixer(h, w_in, gla_a2, gla_a_b, gla_norm_w, ml_conv_w, ml_conv_b, ml_i_b, ml_f_b,
                 s5_lam_re, s5_lam_im, s5_log_step, s5_b_re, s5_b_im, s5_c_re, s5_c_im, s5_d,
                 s5_glu_w, s5_glu_b, proj_gla, proj_ml, proj_s5, branch_gate_b, w_out):
    bsz, t, _ = h.shape
    z = h @ w_in
    gq, gk, gv, ga, gr, mq, mk, mv, mi, mf, mo, su, zg = _split(z, IN_SIZES)

    g_log = jax.nn.log_sigmoid((ga @ gla_a2 + gla_a_b).astype(jnp.float32)) / GLA_GATE_NORM
    o_gla = gla_attention(_heads(gq, GLA_HEADS), _heads(gk, GLA_HEADS), _heads(gv, GLA_HEADS),
                          _heads(g_log, GLA_HEADS))
    o_gla = rms_norm(o_gla, gla_norm_w.reshape(GLA_HEADS, GLA_DV)).reshape(bsz, t, -1)
    y_gla = o_gla * jax.nn.silu(gr)

    qk = jax.nn.silu(causal_dwconv(jnp.concatenate([mq, mk], axis=-1), ml_conv_w, ml_conv_b))
    mq = qk[..., :ML_HEADS * ML_DK]
    mk = qk[..., ML_HEADS * ML_DK:]
    h_ml = mlstm_memory(_heads(mq, ML_HEADS), _heads(mk, ML_HEADS), _heads(mv, ML_HEADS),
                        mi + ml_i_b, mf + ml_f_b)
    y_ml = jax.nn.sigmoid(mo) * h_ml.reshape(bsz, t, -1)

    y_s5 = jax.nn.gelu(s5_ssm(su, s5_lam_re, s5_lam_im, s5_log_step, s5_b_re, s5_b_im,
                              s5_c_re, s5_c_im, s5_d))
    y_s5 = y_s5 * jax.nn.sigmoid(y_s5 @ s5_glu_w + s5_glu_b)

    gates = jax.nn.sigmoid(zg + branch_gate_b).reshape(bsz, t, N_BRANCH, D_MODEL)
    merged = (gates[:, :, 0] * (y_gla @ proj_gla)
              + gates[:, :, 1] * (y_ml @ proj_ml)
              + gates[:, :, 2] * (y_s5 @ proj_s5))
    return merged @ w_out


def swiglu_ffn(h, w_in, w_out):
    gate, up = jnp.split(h @ w_in, 2, axis=-1)
    return (jax.nn.silu(gate) * up) @ w_out


def setup_inputs(seed: int = 0) -> dict:
    key = jax.random.key(seed)
    keys = list(jax.random.split(key, 48))

    def normal(shape, scale):
        return scale * jax.random.normal(keys.pop(), shape, jnp.float32)

    def uniform(shape):
        return jax.random.uniform(keys.pop(), shape, jnp.float32)

    def gain(shape):
        return 1.0 + normal(shape, 0.05)

    L, D = DEPTH, D_MODEL
    glw = GLA_HEADS * GLA_DV
    mlw = ML_HEADS * ML_DV
    n_idx = jnp.arange(S5_STATE, dtype=jnp.float32)
    log_lo, log_hi = math.log(S5_DT_MIN), math.log(S5_DT_MAX)
    return {
        'x': normal((BATCH, SEQ, D), 1.0),
        'c': normal((BATCH, D), 1.0),
        'ada_w': normal((L, D, 6 * D), 0.1 * D ** -0.5),
        'ada_b': normal((L, 6 * D), 0.01),
        'pre1_w': gain((L, D)),
        'post1_w': gain((L, D)),
        'pre2_w': gain((L, D)),
        'post2_w': gain((L, D)),
        'w_in': normal((L, D, IN_DIM), D ** -0.5),
        'gla_a2': normal((L, GLA_RANK, GLA_HEADS * GLA_DK), GLA_RANK ** -0.5),
        'gla_a_b': normal((L, GLA_HEADS * GLA_DK), 0.1),
        'gla_norm_w': gain((L, glw)),
        'ml_conv_w': normal((L, ML_CONV, 2 * ML_HEADS * ML_DK), ML_CONV ** -0.5),
        'ml_conv_b': normal((L, 2 * ML_HEADS * ML_DK), 0.01),
        'ml_i_b': normal((L, ML_HEADS), 0.1),
        'ml_f_b': 3.0 + 3.0 * uniform((L, ML_HEADS)),
        's5_lam_re': -0.5 + normal((L, S5_GROUPS, S5_STATE), 0.01),
        's5_lam_im': math.pi * n_idx + normal((L, S5_GROUPS, S5_STATE), 0.01),
        's5_log_step': log_lo + (log_hi - log_lo) * uniform((L, S5_GROUPS)),
        's5_b_re': normal((L, S5_GROUPS, S5_STATE, S5_GROUP), (2 * S5_GROUP) ** -0.5),
        's5_b_im': normal((L, S5_GROUPS, S5_STATE, S5_GROUP), (2 * S5_GROUP) ** -0.5),
        's5_c_re': normal((L, S5_GROUPS, S5_GROUP, S5_STATE), S5_STATE ** -0.5),
        's5_c_im': normal((L, S5_GROUPS, S5_GROUP, S5_STATE), S5_STATE ** -0.5),
        's5_d': normal((L, S5_WIDTH), 1.0),
        's5_glu_w': normal((L, S5_WIDTH, S5_WIDTH), S5_WIDTH ** -0.5),
        's5_glu_b': normal((L, S5_WIDTH), 0.01),
        'proj_gla': normal((L, glw, D), glw ** -0.5),
        'proj_ml': normal((L, mlw, D), mlw ** -0.5),
        'proj_s5': normal((L, S5_WIDTH, D), S5_WIDTH ** -0.5),
        'branch_gate_b': normal((L, N_BRANCH * D), 0.01),
        'w_out': normal((L, D, D), D ** -0.5),
        'ffn_w_in': normal((L, D, 2 * FFN_HIDDEN), D ** -0.5),
        'ffn_w_out': normal((L, FFN_HIDDEN, D), FFN_HIDDEN ** -0.5),
    }


def reference(x, c, ada_w, ada_b, pre1_w, post1_w, pre2_w, post2_w, w_in, gla_a2, gla_a_b,
              gla_norm_w, ml_conv_w, ml_conv_b, ml_i_b, ml_f_b, s5_lam_re, s5_lam_im, s5_log_step,
              s5_b_re, s5_b_im, s5_c_re, s5_c_im, s5_d, s5_glu_w, s5_glu_b, proj_gla, proj_ml,
              proj_s5, branch_gate_b, w_out, ffn_w_in, ffn_w_out):
    c_act = jax.nn.silu(c)
    for l in range(DEPTH):
        mod = c_act @ ada_w[l] + ada_b[l]
        sh1, sc1, gt1, sh2, sc2, gt2 = [m[:, None, :] for m in jnp.split(mod, 6, axis=-1)]

        h = rms_norm(x, pre1_w[l]) * (1.0 + sc1) + sh1
        y = hybrid_mixer(h, w_in[l], gla_a2[l], gla_a_b[l], gla_norm_w[l], ml_conv_w[l], ml_conv_b[l],
                         ml_i_b[l], ml_f_b[l], s5_lam_re[l], s5_lam_im[l], s5_log_step[l],
                         s5_b_re[l], s5_b_im[l], s5_c_re[l], s5_c_im[l], s5_d[l], s5_glu_w[l],
                         s5_glu_b[l], proj_gla[l], proj_ml[l], proj_s5[l], branch_gate_b[l], w_out[l])
        x = x + (1.0 + gt1) * rms_norm(y, post1_w[l])

        h = rms_norm(x, pre2_w[l]) * (1.0 + sc2) + sh2
        y = swiglu_ffn(h, ffn_w_in[l], ffn_w_out[l])
        x = x + (1.0 + gt2) * rms_norm(y, post2_w[l])
    return x
```

```python
import math
import numpy as np
from contextlib import ExitStack
import concourse.bass as bass
import concourse.mybir as mybir
from concourse.bass_utils import run_bass_kernel_spmd

F32 = mybir.dt.float32
BF16 = mybir.dt.bfloat16
AF = mybir.ActivationFunctionType
ALU = mybir.AluOpType
AX = mybir.AxisListType

D = 1024
IN_DIM = 6680
FH = 2816
C_GQ, C_GK, C_GV, C_GA, C_GR = 0, 256, 512, 1024, 1040
C_MQ, C_MK, C_MV, C_MI, C_MF, C_MO = 1552, 1808, 2064, 2576, 2580, 2584
C_SU, C_ZG = 3096, 3608
EPS = 1e-6
ST = 512
CH = 128
SL = 64


class Buf:
    __slots__ = ("name", "w", "r")

    def __init__(self, name):
        self.name = name
        self.w = {}
        self.r = {}


class TT:
    def __init__(self, t, name):
        self.t = t
        self.b = Buf(name)

    def __getitem__(self, k):
        return self.t[k]


class FW:
    def __init__(self, nc, es, ndma=12):
        self.nc = nc
        self.eng = {"pe": nc.tensor, "act": nc.scalar, "dve": nc.vector, "pool": nc.gpsimd, "sp": nc.sync}
        self.sem = {}
        self.cnt = {}
        self.seen = {}
        for n in self.eng:
            self.sem[n] = es.enter_context(nc.semaphore("s_" + n))
            self.cnt[n] = 0
            self.seen[n] = {}
        self.dq = {}
        for q in ("sp", "pool"):
            slots = []
            for i in range(ndma):
                key = "d_%s%d" % (q, i)
                self.sem[key] = es.enter_context(nc.semaphore(key))
                slots.append(key)
            self.dq[q] = {"slots": slots, "n": 0, "val": {k: 0 for k in slots}}

    def _wait(self, e, key, val):
        if val <= 0 or self.seen[e].get(key, 0) >= val:
            return
        self.eng[e].wait_ge(self.sem[key], val)
        self.seen[e][key] = val

    def _deps(self, e, reads, writes):
        need = {}
        for t in reads:
            for k, v in t.b.w.items():
                if need.get(k, 0) < v:
                    need[k] = v
        for t in writes:
            for k, v in t.b.w.items():
                if need.get(k, 0) < v:
                    need[k] = v
            for k, v in t.b.r.items():
                if need.get(k, 0) < v:
                    need[k] = v
        for k, v in need.items():
            if e == "pe" and k == "pe":
                continue
            self._wait(e, k, v)

    def op(self, e, fn, r=(), w=(), pw=()):
        self._deps(e, r, tuple(w) + tuple(pw))
        ins = fn(self.eng[e])
        self.cnt[e] += 1
        ins.then_inc(self.sem[e], 1)
        c = self.cnt[e]
        for t in r:
            t.b.r[e] = c
        for t in w:
            t.b.w = {e: c}
            t.b.r = {}
        for t in pw:
            t.b.w[e] = c
        return ins

    def dma(self, q, out, in_, r=(), w=(), pw=(), **kw):
        d = self.dq[q]
        key = d["slots"][d["n"] % len(d["slots"])]
        d["n"] += 1
        self._wait(q, key, d["val"][key])
        self._deps(q, r, tuple(w) + tuple(pw))
        ins = self.eng[q].dma_start(out=out, in_=in_, **kw)
        d["val"][key] += 16
        ins.then_inc(self.sem[key], 16)
        v = d["val"][key]
        for t in r:
            t.b.r[key] = v
        for t in w:
            t.b.w = {key: v}
            t.b.r = {}
        for t in pw:
            t.b.w[key] = v
        return ins

    def barrier(self):
        targets = {n: self.cnt[n] for n in self.eng}
        for q, d in self.dq.items():
            for k, v in d["val"].items():
                targets[k] = v
        for e in self.eng:
            for k, v in targets.items():
                if e == "pe" and k == "pe":
                    continue
                self._wait(e, k, v)


class Prog:
    def __init__(self, T, L, debug=False):
        self.T = T
        self.L = L
        self.debug = debug
        self.NST = T // ST
        self.NCH = T // CH
        self.NTH = T // 8
        self.NSC = T // SL
        self.nc = bass.Bass("TRN2", target_bir_lowering=False)

    def dram_in(self, name, shape, dt=F32):
        return self.nc.dram_tensor(name, list(shape), dt, kind="ExternalInput").ap()

    def dram_scr(self, name, shape, dt):
        kind = "ExternalOutput" if self.debug else "Internal"
        t = TT(self.nc.dram_tensor(name, list(shape), dt, kind=kind).ap(), name)
        return t

    def sb(self, es, name, shape, dt):
        self._uid = getattr(self, "_uid", 0) + 1
        nm = "sb%d_%s" % (self._uid, name)
        return TT(es.enter_context(self.nc.sbuf_tensor(nm, list(shape), dt)), nm)

    def build(self):
        nc = self.nc
        T, L = self.T, self.L
        es = ExitStack()
        with es:
            self.fw = FW(nc, es)
            self.declare_io()
            self.PS = [TT(es.enter_context(nc.psum_tensor("ps%d" % i, [128, 512], F32)), "ps%d" % i) for i in range(8)]
            self.consts(es)
            self.prep_alloc(es)
            import os
            ph = os.environ.get("PHASES", "AS1BF")
            with ExitStack() as pes0:
                wt0 = [self.sb(pes0, "adaw%d" % i, [128, 8, 128], F32) for i in range(3)]
                for stp in self.prep_steps(0, wt0, self.PS[7]):
                    stp()
                self.fw.barrier()
            for l in range(L):
                self.set_layer(l)
                if "A" in ph:
                    self.phase_A(l)
                    self.fw.barrier()
                if "S" in ph:
                    self.phase_S5(l)
                    self.fw.barrier()
                if "1" in ph:
                    self.phase_B1(l)
                    self.fw.barrier()
                if "B" in ph:
                    self.phase_B2(l)
                    self.fw.barrier()
                if "F" in ph:
                    self.phase_F(l)
                    self.fw.barrier()
            self.fw.barrier()
        return nc

    def declare_io(self):
        T, L = self.T, self.L
        di = self.dram_in
        self.xT_in = di("xT", [D, T])
        self.cT = di("cT", [128, 8])
        self.ada_w = di("ada_w", [L, D, 6 * D])
        self.ada_bT = di("ada_bT", [L, 128, 48])
        self.vecs = di("vecs", [L, 128, 4, 8])
        self.w_in = di("w_in", [L, D, IN_DIM])
        self.gla_a2 = di("gla_a2", [L, 16, 256])
        self.gla_ab = di("gla_ab", [L, 128, 2])
        self.gla_nw = di("gla_nw", [L, 128, 4])
        self.conv_w = di("conv_w", [L, 128, 4, 4])
        self.conv_b = di("conv_b", [L, 128, 4])
        self.ml_ifb = di("ml_ifb", [L, 4, 2])
        self.s5_lre = di("s5_lre", [L, 128, 32])
        self.s5_lim = di("s5_lim", [L, 128, 32])
        self.s5_ls = di("s5_ls", [L, 128, 32])
        self.s5_bre = di("s5_bre", [L, 128, 32, 16])
        self.s5_bim = di("s5_bim", [L, 128, 32, 16])
        self.s5_cre = di("s5_cre", [L, 128, 32, 16])
        self.s5_cim = di("s5_cim", [L, 128, 32, 16])
        self.s5_dblk = di("s5_dblk", [L, 128, 32])
        self.glu_w = di("glu_w", [L, 512, 512])
        self.glu_b = di("glu_b", [L, 128, 4])
        self.proj = [di("proj_gla", [L, 512, D]), di("proj_ml", [L, 512, D]), di("proj_s5", [L, 512, D])]
        self.bgb = di("bgb", [L, 128, 24])
        self.w_out = di("w_out", [L, D, D])
        self.ffn_w1 = di("ffn_w1", [L, D, 2 * FH])
        self.ffn_w2 = di("ffn_w2", [L, FH, D])
        self.xT = TT(self.nc.dram_tensor("outT", [D, T], F32, kind="ExternalOutput").ap(), "outT")
        ds = self.dram_scr
        self.s_gq = ds("s_gq", [256, T], BF16)
        self.s_gk = ds("s_gk", [256, T], BF16)
        self.s_g = ds("s_g", [256, T], F32)
        self.s_gr = ds("s_gr", [512, T], BF16)
        self.s_mqk = ds("s_mqk", [512, T], BF16)
        self.s_mi = ds("s_mi", [4, T], F32)
        self.s_mf = ds("s_mf", [4, T], F32)
        self.s_mo = ds("s_mo", [512, T], BF16)
        self.s_gates = ds("s_gates", [3072, T], BF16)
        self.s_gv = ds("s_gv", [T, 512], BF16)
        self.s_mv = ds("s_mv", [T, 512], BF16)
        self.s_ublk = ds("s_ublk", [32, 128, T // 8], BF16)
        self.s_ys5 = ds("s_ys5", [512, T], BF16)
        self.s_kfac = ds("s_kfac", [4, T], F32)
        self.s_floor = ds("s_floor", [4, T], F32)
        if self.debug:
            self.s_ygla = ds("s_ygla", [512, T], BF16)
            self.s_yml = ds("s_yml", [512, T], BF16)
            self.s_x1 = ds("s_x1", [D, T], F32)

    def consts(self, es):
        nc, fw = self.nc, self.fw
        sb = self.sb
        self.identf = sb(es, "identf", [128, 128], F32)
        self.ident = sb(es, "ident", [128, 128], BF16)
        self.ones_bf = sb(es, "ones_bf", [128, 128], BF16)
        self.maskT = sb(es, "maskT", [128, 128], F32)
        self.mask01 = sb(es, "mask01", [128, ST], F32)
        fw.op("pool", lambda e: e.memset(self.identf[:], 0.0), w=[self.identf])
        fw.op("pool", lambda e: e.affine_select(out=self.identf[:], in_=self.identf[:], pattern=[[-1, 128]],
                                                compare_op=ALU.not_equal, fill=1.0, base=0, channel_multiplier=1),
              r=[self.identf], w=[self.identf])
        fw.op("dve", lambda e: e.tensor_copy(out=self.ident[:], in_=self.identf[:]), r=[self.identf], w=[self.ident])
        fw.op("dve", lambda e: e.memset(self.ones_bf[:], 1.0), w=[self.ones_bf])
        fw.op("pool", lambda e: e.memset(self.maskT[:], 1.0), w=[self.maskT])
        fw.op("pool", lambda e: e.affine_select(out=self.maskT[:], in_=self.maskT[:], pattern=[[1, 128]],
                                                compare_op=ALU.is_ge, fill=0.0, base=0, channel_multiplier=-1),
              r=[self.maskT], w=[self.maskT])
        fw.op("dve", lambda e: e.memset(self.mask01[:], 1.0), w=[self.mask01])
        fw.op("dve", lambda e: e.memset(self.mask01[:].rearrange("p (c t) -> p c t", t=CH)[:, :, 0:1], 0.0),
              r=[self.mask01], w=[self.mask01])
        self.selk = []
        self.selv = []
        for hp in range(2):
            t = sb(es, "selk%d" % hp, [4, 128], F32)
            fw.op("pool", lambda e, t=t: e.memset(t[:], 1.0), w=[t])
            fw.op("pool", lambda e, t=t, hp=hp: e.affine_select(out=t[:], in_=t[:], pattern=[[1, 128]], compare_op=ALU.is_ge,
                                                              fill=0.0, base=128 * hp, channel_multiplier=-64), r=[t], w=[t])
            fw.op("pool", lambda e, t=t, hp=hp: e.affine_select(out=t[:], in_=t[:], pattern=[[-1, 128]], compare_op=ALU.is_ge,
                                                              fill=0.0, base=63 - 128 * hp, channel_multiplier=64), r=[t], w=[t])
            self.selk.append(t)
        for h in range(4):
            t = sb(es, "selv%d" % h, [4, 128], F32)
            fw.op("pool", lambda e, t=t: e.memset(t[:], 1.0), w=[t])
            fw.op("pool", lambda e, t=t, h=h: e.affine_select(out=t[:], in_=t[:], pattern=[[0, 128]], compare_op=ALU.is_equal,
                                                            fill=0.0, base=-h, channel_multiplier=1), r=[t], w=[t])
            self.selv.append(t)
        self.eps_t = sb(es, "eps_t", [128, 1], F32)
        fw.op("dve", lambda e: e.memset(self.eps_t[:], EPS), w=[self.eps_t])
        self.nrm_tmp = sb(es, "nrm_tmp", [128, ST], F32)
        self.ln8 = sb(es, "ln8", [128, 1], F32)
        fw.op("dve", lambda e: e.memset(self.ln8[:], math.log(0.125)), w=[self.ln8])
        self.cact = sb(es, "cact", [128, 8], F32)
        fw.dma("sp", self.cact[:], self.cT[:, :], w=[self.cact])
        fw.op("act", lambda e: e.activation(out=self.cact[:], in_=self.cact[:], func=AF.Silu), r=[self.cact], w=[self.cact])
        nchunk = 8
        rows = D // nchunk
        for i in range(nchunk):
            fw.dma("sp", self.xT[i * rows:(i + 1) * rows, :], self.xT_in[i * rows:(i + 1) * rows, :], pw=[self.xT])

    def prep_alloc(self, es):
        sb = self.sb
        self.LP = []
        for i in range(2):
            d = {}
            d["mod"] = sb(es, "mod%d" % i, [128, 48], F32)
            for k in ("G1", "G2", "GT1", "GT2"):
                d[k] = sb(es, "%s_%d" % (k, i), [128, 8], F32)
            d["vec"] = sb(es, "vec%d" % i, [128, 4, 8], F32)
            d["adab"] = sb(es, "adab%d" % i, [128, 48], F32)
            self.LP.append(d)
        self.decay = sb(es, "decay", [4, self.NCH], F32)

    def prep_steps(self, l, wt, ps):
        fw = self.fw
        d = self.LP[l % 2]
        mod, vec, adab = d["mod"], d["vec"], d["adab"]

        def first():
            fw.dma("sp", vec[:], self.vecs[l], w=[vec])
            fw.dma("sp", adab[:], self.ada_bT[l], w=[adab])

        def block(j):
            t = wt[j % len(wt)]
            fw.dma("sp", t[:], self.ada_w[l, :, j * 128:(j + 1) * 128].rearrange("(kc p) m -> p kc m", p=128), w=[t])
            for kc in range(8):
                fw.op("pe", lambda e, kc=kc: e.matmul(ps[:, j:j + 1], lhsT=t[:, kc, :], rhs=self.cact[:, kc:kc + 1], start=(kc == 0), stop=(kc == 7)),
                      r=[t, self.cact], pw=[ps])

        def final():
            fw.op("dve", lambda e: e.tensor_tensor(out=mod[:], in0=ps[:, 0:48], in1=adab[:], op=ALU.add), r=[ps, adab], w=[mod])
            for (k, piece, vi) in (("G1", 1, 0), ("GT1", 2, 1), ("G2", 4, 2), ("GT2", 5, 3)):
                out = d[k]
                fw.op("dve", lambda e, out=out, piece=piece, vi=vi: e.scalar_tensor_tensor(out=out[:], in0=mod[:, piece * 8:(piece + 1) * 8], scalar=1.0, in1=vec[:, vi, :],
                                                                                       op0=ALU.add, op1=ALU.mult), r=[mod, vec], w=[out])
        steps = [first] + [(lambda j=j: block(j)) for j in range(48)] + [final]
        return steps

    def set_layer(self, l):
        d = self.LP[l % 2]
        self.mod = d["mod"]
        self.G1, self.G2, self.GT1, self.GT2 = d["G1"], d["G2"], d["GT1"], d["GT2"]
        mod = self.mod
        self.SH1 = lambda kc: mod[:, 0 + kc:0 + kc + 1]
        self.SH2 = lambda kc: mod[:, 24 + kc:24 + kc + 1]

    def prenorm(self, xs, hT, sq, rstd, G, SH, ps_ss, W=ST):
        fw = self.fw
        fw.op("act", lambda e: e.activation(out=sq[:], in_=xs[:], func=AF.Square), r=[xs], w=[sq])
        for kc in range(8):
            fw.op("pe", lambda e, kc=kc: e.matmul(ps_ss[:, 0:W], lhsT=self.ones_bf[:], rhs=sq[:, kc, :], start=(kc == 0), stop=(kc == 7)),
                  r=[self.ones_bf, sq], pw=[ps_ss])
        fw.op("act", lambda e: e.activation(out=rstd[:], in_=ps_ss[:, 0:W], func=AF.Ln, scale=1.0 / D, bias=self.eps_t[:, 0:1]), r=[ps_ss, self.eps_t], w=[rstd])
        fw.op("act", lambda e: e.activation(out=rstd[:], in_=rstd[:], func=AF.Exp, scale=-0.5), r=[rstd], w=[rstd])
        for kc in range(8):
            fw.op("dve", lambda e, kc=kc: e.scalar_tensor_tensor(out=self.nrm_tmp[:, 0:W], in0=xs[:, kc, :], scalar=G[:, kc:kc + 1], in1=rstd[:],
                                                               op0=ALU.mult, op1=ALU.mult), r=[xs, G, rstd], w=[self.nrm_tmp])
            fw.op("act", lambda e, kc=kc: e.activation(out=hT[:, kc, :], in_=self.nrm_tmp[:, 0:W], func=AF.Identity, bias=SH(kc), scale=1.0),
                  r=[self.nrm_tmp, self.mod], pw=[hT])

    def phase_A(self, l):
        nc, fw, sb = self.nc, self.fw, self.sb
        PS = self.PS
        T = self.T
        with ExitStack() as pes:
            wsb_t = sb(pes, "w_in_sb", [128, 8, IN_DIM], BF16)
            bounds = [0, 2584, 4632, 6680]
            wgroups = [(bounds[i], bounds[i + 1], TT(wsb_t.t, "wg%d" % i)) for i in range(len(bounds) - 1)]

            def wg(c0, M):
                for (a, b_, v) in wgroups:
                    if a <= c0 and c0 + M <= b_:
                        return v
                raise AssertionError("block crosses weight group")

            for gi in range(3):
                a, b_, v = wgroups[gi]
                for kc in range(8):
                    fw.dma("pool", wsb_t[:, kc, a:b_], self.w_in[l, kc * 128:(kc + 1) * 128, a:b_], pw=[v])
            wsb = wsb_t
            a2 = sb(pes, "a2", [16, 256], F32)
            ab = sb(pes, "ab", [128, 2], F32)
            nab = sb(pes, "nab", [128, 2], F32)
            bgb = sb(pes, "bgb", [128, 24], F32)
            fw.dma("sp", a2[:], self.gla_a2[l], w=[a2])
            fw.dma("sp", ab[:], self.gla_ab[l], w=[ab])
            fw.dma("sp", bgb[:], self.bgb[l], w=[bgb])
            fw.op("dve", lambda e: e.tensor_scalar(out=nab[:], in0=ab[:], scalar1=-1.0, scalar2=None, op0=ALU.mult), r=[ab], w=[nab])
            xs = sb(pes, "xs", [128, 8, ST], F32)
            hTs = [sb(pes, "hT%d" % i, [128, 8, ST], BF16) for i in range(2)]
            sq = sb(pes, "sq", [128, 8, ST], BF16)
            rstd = sb(pes, "rstd", [128, ST], F32)
            stg = [sb(pes, "stg%d" % i, [128, ST], BF16) for i in range(6)]
            stgf = [sb(pes, "stgf%d" % i, [128, ST], F32) for i in range(3)]
            z8 = sb(pes, "z8", [64, 32, 128], BF16)
            ubk = sb(pes, "ubk", [128, 32, 64], BF16)
            gaT = sb(pes, "gaT", [16, ST], F32)
            cnt = {"s": 0, "f": 0, "p": 0}

            def nstg():
                cnt["s"] += 1
                return stg[cnt["s"] % 6]

            def nstgf():
                cnt["f"] += 1
                return stgf[cnt["f"] % 3]

            def nps():
                cnt["p"] += 1
                return PS[cnt["p"] % 4]

            cur = {}

            def fm_block(c0, M):
                ps = nps()
                hT = cur["hT"]
                v = wg(c0, M)
                for kc in range(8):
                    fw.op("pe", lambda e, kc=kc: e.matmul(ps[0:M, :], lhsT=wsb[:, kc, c0:c0 + M], rhs=hT[:, kc, :], start=(kc == 0), stop=(kc == 7)),
                          r=[v, hT], pw=[ps])
                return ps

            def do_prenorm(st_):
                fw.dma("sp", xs[:], self.xT[:, st_ * ST:(st_ + 1) * ST].rearrange("(kc p) t -> p kc t", p=128), r=[self.xT], w=[xs])
                self.prenorm(xs, hTs[st_ % 2], sq, rstd, self.G1, self.SH1, PS[4])

            do_prenorm(0)
            for st in range(self.NST):
                t0 = st * ST
                hT = hTs[st % 2]
                cur["hT"] = hT
                for (c0, dst, r0) in [(C_GQ, self.s_gq, 0), (C_GQ + 128, self.s_gq, 128), (C_GK, self.s_gk, 0), (C_GK + 128, self.s_gk, 128),
                                      (C_MQ, self.s_mqk, 0), (C_MQ + 128, self.s_mqk, 128), (C_MK, self.s_mqk, 256), (C_MK + 128, self.s_mqk, 384)]:
                    ps = fm_block(c0, 128)
                    s = nstg()
                    fw.op("dve", lambda e, s=s, ps=ps: e.tensor_copy(out=s[:], in_=ps[:]), r=[ps], w=[s])
                    fw.dma("pool", dst[r0:r0 + 128, t0:t0 + ST], s[:], r=[s], pw=[dst])
                if st + 1 < self.NST:
                    do_prenorm(st + 1)
                for (c0, dst) in [(C_MI, self.s_mi), (C_MF, self.s_mf)]:
                    ps = fm_block(c0, 4)
                    s = nstgf()
                    fw.op("dve", lambda e, s=s, ps=ps: e.tensor_copy(out=s[0:4, :], in_=ps[0:4, :]), r=[ps], w=[s])
                    fw.dma("pool", dst[:, t0:t0 + ST], s[0:4, :], r=[s], pw=[dst])
                ps = fm_block(C_GA, 16)
                fw.op("dve", lambda e, ps=ps: e.tensor_copy(out=gaT[:], in_=ps[0:16, :]), r=[ps], w=[gaT])
                for hp in range(2):
                    ps = nps()
                    fw.op("pe", lambda e, ps=ps, hp=hp: e.matmul(ps[:], lhsT=a2[:, hp * 128:(hp + 1) * 128], rhs=gaT[:], start=True, stop=True),
                          r=[a2, gaT], w=[ps])
                    s = nstgf()
                    fw.op("act", lambda e, ps=ps, s=s, hp=hp: e.activation(out=s[:], in_=ps[:], func=AF.Exp, scale=-1.0, bias=nab[:, hp:hp + 1]),
                          r=[ps, nab], w=[s])
                    fw.op("act", lambda e, s=s: e.activation(out=s[:], in_=s[:], func=AF.Ln, bias=1.0), r=[s], w=[s])
                    fw.op("dve", lambda e, s=s: e.tensor_scalar(out=s[:], in0=s[:], scalar1=-1.0 / 16.0, scalar2=None, op0=ALU.mult), r=[s], w=[s])
                    fw.dma("pool", self.s_g[hp * 128:(hp + 1) * 128, t0:t0 + ST], s[:], r=[s], pw=[self.s_g])
                for j in range(4):
                    ps = fm_block(C_GR + j * 128, 128)
                    s = nstg()
                    fw.op("act", lambda e, s=s, ps=ps: e.activation(out=s[:], in_=ps[:], func=AF.Silu), r=[ps], w=[s])
                    fw.dma("pool", self.s_gr[j * 128:(j + 1) * 128, t0:t0 + ST], s[:], r=[s], pw=[self.s_gr])
                for j in range(4):
                    ps = fm_block(C_MO + j * 128, 128)
                    s = nstg()
                    fw.op("act", lambda e, s=s, ps=ps: e.activation(out=s[:], in_=ps[:], func=AF.Sigmoid), r=[ps], w=[s])
                    fw.dma("pool", self.s_mo[j * 128:(j + 1) * 128, t0:t0 + ST], s[:], r=[s], pw=[self.s_mo])
                for j in range(24):
                    ps = fm_block(C_ZG + j * 128, 128)
                    s = nstg()
                    fw.op("act", lambda e, s=s, ps=ps, j=j: e.activation(out=s[:], in_=ps[:], func=AF.Sigmoid, bias=bgb[:, j:j + 1]), r=[ps, bgb], w=[s])
                    fw.dma("pool", self.s_gates[j * 128:(j + 1) * 128, t0:t0 + ST], s[:], r=[s], pw=[self.s_gates])
                for (c0, dst) in [(C_GV, self.s_gv), (C_MV, self.s_mv)]:
                    for sub in range(4):
                        ps = nps()
                        for kc in range(8):
                            fw.op("pe", lambda e, kc=kc, ps=ps, sub=sub, c0=c0: e.matmul(ps[:], lhsT=hT[:, kc, sub * 128:(sub + 1) * 128], rhs=wsb[:, kc, c0:c0 + 512],
                                                                                 start=(kc == 0), stop=(kc == 7)), r=[wg(c0, 512), hT], pw=[ps])
                        s = nstg()
                        fw.op("dve", lambda e, s=s, ps=ps: e.tensor_copy(out=s[:], in_=ps[:]), r=[ps], w=[s])
                        fw.dma("pool", dst[t0 + sub * 128:t0 + (sub + 1) * 128, :], s[:], r=[s], pw=[dst])
                for jl in range(8):
                    ps = nps()
                    for kc in range(8):
                        fw.op("pe", lambda e, kc=kc, ps=ps, jl=jl: e.matmul(ps[0:64, :], lhsT=hT[:, kc, jl::8], rhs=wsb[:, kc, C_SU:C_SU + 512],
                                                                        start=(kc == 0), stop=(kc == 7)), r=[wg(C_SU, 512), hT], pw=[ps])
                    fw.op("act", lambda e, ps=ps, jl=jl: e.activation(out=z8[:, :, jl * 16:(jl + 1) * 16], in_=ps[0:64, :].rearrange("p (g c) -> p g c", c=16), func=AF.Copy), r=[ps], pw=[z8])
                for gb in range(2):
                    pst = PS[5 + gb]
                    pv = pst[:].bitcast(BF16)
                    for gg in range(16):
                        g = gb * 16 + gg
                        fw.op("pe", lambda e, g=g, gg=gg, pv=pv: e.transpose(pv[:, gg * 64:(gg + 1) * 64], z8[:, g, :], self.ident[0:64, 0:64]),
                              r=[z8, self.ident], pw=[pst])
                    fw.op("dve", lambda e, gb=gb, pv=pv: e.tensor_copy(out=ubk[:, gb * 16:(gb + 1) * 16, :], in_=pv.rearrange("p (g t) -> p g t", t=64)),
                          r=[pst], pw=[ubk])
                th0 = st * (ST // 8)
                fw.dma("pool", self.s_ublk[:, :, th0:th0 + ST // 8].rearrange("g p t -> p g t"), ubk[:], r=[ubk], pw=[self.s_ublk])

    def postnorm(self, xs, ysb, sq, rstd, GT, ps_ss, W):
        fw = self.fw
        for kc in range(8):
            fw.op("pe", lambda e, kc=kc: e.matmul(ps_ss[:, 0:W], lhsT=self.ones_bf[:], rhs=sq[:, kc, :], start=(kc == 0), stop=(kc == 7)),
                  r=[self.ones_bf, sq], pw=[ps_ss])
        fw.op("act", lambda e: e.activation(out=rstd[:], in_=ps_ss[:, 0:W], func=AF.Ln, scale=1.0 / D, bias=self.eps_t[:, 0:1]), r=[ps_ss, self.eps_t], w=[rstd])
        fw.op("act", lambda e: e.activation(out=rstd[:], in_=rstd[:], func=AF.Exp, scale=-0.5), r=[rstd], w=[rstd])
        for kc in range(8):
            fw.op("dve", lambda e, kc=kc: e.tensor_tensor(out=ysb[:, kc, :], in0=ysb[:, kc, :], in1=rstd[:], op=ALU.mult), r=[ysb, rstd], pw=[ysb])
            fw.op("dve", lambda e, kc=kc: e.scalar_tensor_tensor(out=xs[:, kc, :], in0=ysb[:, kc, :], scalar=GT[:, kc:kc + 1], in1=xs[:, kc, :],
                                                               op0=ALU.mult, op1=ALU.add), r=[ysb, GT, xs], pw=[xs])

    def phase_S5(self, l):
        nc, fw, sb = self.nc, self.fw, self.sb
        PS = self.PS
        T, NTH, NSC = self.T, self.NTH, self.NSC
        NT8 = NTH // 128
        MAGIC = 12582912.0
        TWO_PI = 6.28318
        import os
        STOP = int(os.environ.get("S5STOP", "99"))
        with ExitStack() as pes:
            PWre = sb(pes, "PWre", [128, 32, 72], F32)
            PWim = sb(pes, "PWim", [128, 32, 72], F32)
            BA = sb(pes, "BA", [128, 32, 16], F32)
            BB = sb(pes, "BB", [128, 32, 16], F32)
            CA = sb(pes, "CA", [128, 32, 16], F32)
            CB = sb(pes, "CB", [128, 32, 16], F32)
            AR = sb(pes, "AR", [128, 32], F32)
            AIst = sb(pes, "AIst", [128, 32], F32)
            AIsw = sb(pes, "AIsw", [128, 32], F32)
            dblk = sb(pes, "dblk", [128, 32], F32)
            mask8 = sb(pes, "mask8", [128, 128], F32)
            F7 = sb(pes, "F7", [128, 32, 128], BF16)
            Xprev = sb(pes, "Xprev", [128, 32, NSC], BF16)
            fw.dma("sp", dblk[:], self.s5_dblk[l], w=[dblk])
            fw.op("pool", lambda e: e.memset(mask8[:], 1.0), w=[mask8])
            fw.op("pool", lambda e: e.affine_select(out=mask8[:].rearrange("p (i c) -> p i c", c=16), in_=mask8[:].rearrange("p (i c) -> p i c", c=16),
                                                    pattern=[[16, 8], [0, 16]], compare_op=ALU.is_ge, fill=0.0, base=15, channel_multiplier=-1),
                  r=[mask8], w=[mask8])
            with ExitStack() as tes:
                lre = sb(tes, "lre", [128, 32], F32)
                lim = sb(tes, "lim", [128, 32], F32)
                dtt = sb(tes, "dtt", [128, 32], F32)
                rho = sb(tes, "rho", [128, 32], F32)
                th = sb(tes, "th", [128, 32], F32)
                kki = sb(tes, "kki", [128, 72], mybir.dt.int32)
                kk = sb(tes, "kk", [128, 72], F32)
                fw.dma("sp", lre[:], self.s5_lre[l], w=[lre])
                fw.dma("sp", lim[:], self.s5_lim[l], w=[lim])
                fw.dma("sp", dtt[:], self.s5_ls[l], w=[dtt])
                fw.op("act", lambda e: e.activation(out=dtt[:], in_=dtt[:], func=AF.Exp), r=[dtt], w=[dtt])
                fw.op("dve", lambda e: e.tensor_scalar(out=lre[:], in0=lre[:], scalar1=-1e-4, scalar2=None, op0=ALU.min), r=[lre], w=[lre])
                fw.op("dve", lambda e: e.tensor_tensor(out=rho[:], in0=lre[:], in1=dtt[:], op=ALU.mult), r=[lre, dtt], w=[rho])
                fw.op("dve", lambda e: e.tensor_tensor(out=th[:], in0=lim[:], in1=dtt[:], op=ALU.mult), r=[lim, dtt], w=[th])
                fw.op("pool", lambda e: e.iota(kki[:], pattern=[[1, 72]], base=-7, channel_multiplier=0), w=[kki])
                fw.op("dve", lambda e: e.tensor_copy(out=kk[:], in_=kki[:]), r=[kki], w=[kk])
                big = [sb(tes, "big%d" % i, [128, 32, 72], F32) for i in range(4)]
                ph, yy, tt_, ee = big
                B3 = lambda a: a[:, :].unsqueeze(2).to_broadcast([128, 32, 72])
                K3 = kk[:, :].unsqueeze(1).to_broadcast([128, 32, 72])
                fw.op("dve", lambda e: e.tensor_tensor(out=ph[:], in0=B3(th), in1=K3, op=ALU.mult), r=[th, kk], w=[ph])
                fw.op("dve", lambda e: e.tensor_tensor(out=ee[:], in0=B3(rho), in1=K3, op=ALU.mult), r=[rho, kk], w=[ee])
                fw.op("act", lambda e: e.activation(out=ee[:], in_=ee[:], func=AF.Exp), r=[ee], w=[ee])
                for (shift, dst) in [(0.0, PWim), (0.25, PWre)]:
                    fw.op("dve", lambda e, shift=shift: e.tensor_scalar(out=yy[:], in0=ph[:], scalar1=1.0 / (2.0 * math.pi), scalar2=shift, op0=ALU.mult, op1=ALU.add),
                          r=[ph], w=[yy])
                    fw.op("dve", lambda e: e.tensor_scalar(out=tt_[:], in0=yy[:], scalar1=MAGIC, scalar2=None, op0=ALU.add), r=[yy], w=[tt_])
                    fw.op("dve", lambda e: e.tensor_scalar(out=tt_[:], in0=tt_[:], scalar1=-MAGIC, scalar2=None, op0=ALU.add), r=[tt_], w=[tt_])
                    fw.op("dve", lambda e: e.tensor_tensor(out=yy[:], in0=yy[:], in1=tt_[:], op=ALU.subtract), r=[yy, tt_], w=[yy])
                    fw.op("act", lambda e: e.activation(out=yy[:], in_=yy[:], func=AF.Sin, scale=TWO_PI), r=[yy], w=[yy])
                    fw.op("dve", lambda e, dst=dst: e.tensor_tensor(out=dst[:], in0=yy[:], in1=ee[:], op=ALU.mult), r=[yy, ee], w=[dst])
                are = PWre[:, :, 8]
                aim = PWim[:, :, 8]
                am1 = sb(tes, "am1", [128, 32], F32)
                inv = sb(tes, "inv", [128, 32], F32)
                t1 = sb(tes, "t1", [128, 32], F32)
                t2 = sb(tes, "t2", [128, 32], F32)
                zre = sb(tes, "zre", [128, 32], F32)
                zim = sb(tes, "zim", [128, 32], F32)
                V = lambda fn, r, w: fw.op("dve", fn, r=r, w=w)
                V(lambda e: e.tensor_scalar(out=am1[:], in0=are, scalar1=-1.0, scalar2=None, op0=ALU.add), [PWre], [am1])
                V(lambda e: e.tensor_tensor(out=t1[:], in0=lre[:], in1=lre[:], op=ALU.mult), [lre], [t1])
                V(lambda e: e.tensor_tensor(out=t2[:], in0=lim[:], in1=lim[:], op=ALU.mult), [lim], [t2])
                V(lambda e: e.tensor_tensor(out=inv[:], in0=t1[:], in1=t2[:], op=ALU.add), [t1, t2], [inv])
                V(lambda e: e.reciprocal(out=inv[:], in_=inv[:]), [inv], [inv])
                V(lambda e: e.tensor_tensor(out=t1[:], in0=am1[:], in1=lre[:], op=ALU.mult), [am1, lre], [t1])
                V(lambda e: e.tensor_tensor(out=t2[:], in0=aim, in1=lim[:], op=ALU.mult), [PWim, lim], [t2])
                V(lambda e: e.tensor_tensor(out=t1[:], in0=t1[:], in1=t2[:], op=ALU.add), [t1, t2], [t1])
                V(lambda e: e.tensor_tensor(out=zre[:], in0=t1[:], in1=inv[:], op=ALU.mult), [t1, inv], [zre])
                V(lambda e: e.tensor_tensor(out=t1[:], in0=aim, in1=lre[:], op=ALU.mult), [PWim, lre], [t1])
                V(lambda e: e.tensor_tensor(out=t2[:], in0=am1[:], in1=lim[:], op=ALU.mult), [am1, lim], [t2])
                V(lambda e: e.tensor_tensor(out=t1[:], in0=t1[:], in1=t2[:], op=ALU.subtract), [t1, t2], [t1])
                V(lambda e: e.tensor_tensor(out=zim[:], in0=t1[:], in1=inv[:], op=ALU.mult), [t1, inv], [zim])
                bre = sb(tes, "bre", [128, 32, 16], F32)
                bim = sb(tes, "bim", [128, 32, 16], F32)
                cre = sb(tes, "cre", [128, 32, 16], F32)
                cim = sb(tes, "cim", [128, 32, 16], F32)
                u1 = sb(tes, "u1", [128, 32, 16], F32)
                u2 = sb(tes, "u2", [128, 32, 16], F32)
                Bre = sb(tes, "Bre", [128, 32, 16], F32)
                Bim = sb(tes, "Bim", [128, 32, 16], F32)
                fw.dma("sp", bre[:], self.s5_bre[l], w=[bre])
                fw.dma("sp", bim[:], self.s5_bim[l], w=[bim])
                fw.dma("sp", cre[:], self.s5_cre[l], w=[cre])
                fw.dma("sp", cim[:], self.s5_cim[l], w=[cim])
                Z3 = lambda a: a[:, :].unsqueeze(2).to_broadcast([128, 32, 16])
                V(lambda e: e.tensor_tensor(out=u1[:], in0=bre[:], in1=Z3(zre), op=ALU.mult), [bre, zre], [u1])
                V(lambda e: e.tensor_tensor(out=u2[:], in0=bim[:], in1=Z3(zim), op=ALU.mult), [bim, zim], [u2])
                V(lambda e: e.tensor_tensor(out=Bre[:], in0=u1[:], in1=u2[:], op=ALU.subtract), [u1, u2], [Bre])
                V(lambda e: e.tensor_tensor(out=u1[:], in0=bim[:], in1=Z3(zre), op=ALU.mult), [bim, zre], [u1])
                V(lambda e: e.tensor_tensor(out=u2[:], in0=bre[:], in1=Z3(zim), op=ALU.mult), [bre, zim], [u2])
                V(lambda e: e.tensor_tensor(out=Bim[:], in0=u1[:], in1=u2[:], op=ALU.add), [u1, u2], [Bim])
                lo, hi = slice(0, 64), slice(64, 128)
                V(lambda e: e.tensor_copy(out=BA[lo], in_=Bre[lo]), [Bre], [BA])
                fw.op("dve", lambda e: e.tensor_copy(out=BA[hi], in_=Bim[hi]), r=[Bim], pw=[BA])
                V(lambda e: e.tensor_scalar(out=BB[lo], in0=Bim[lo], scalar1=-1.0, scalar2=None, op0=ALU.mult), [Bim], [BB])
                fw.op("dve", lambda e: e.tensor_copy(out=BB[hi], in_=Bre[hi]), r=[Bre], pw=[BB])
                V(lambda e: e.tensor_copy(out=CA[lo], in_=cre[lo]), [cre], [CA])
                fw.op("dve", lambda e: e.tensor_scalar(out=CA[hi], in0=cim[hi], scalar1=-1.0, scalar2=None, op0=ALU.mult), r=[cim], pw=[CA])
                V(lambda e: e.tensor_scalar(out=CB[lo], in0=cim[lo], scalar1=-1.0, scalar2=None, op0=ALU.mult), [cim], [CB])
                fw.op("dve", lambda e: e.tensor_scalar(out=CB[hi], in0=cre[hi], scalar1=-1.0, scalar2=None, op0=ALU.mult), r=[cre], pw=[CB])
                V(lambda e: e.tensor_copy(out=AR[:], in_=PWre[:, :, 71]), [PWre], [AR])
                V(lambda e: e.tensor_scalar(out=AIst[lo], in0=PWim[lo, :, 71], scalar1=-1.0, scalar2=None, op0=ALU.mult), [PWim], [AIst])
                fw.op("dve", lambda e: e.tensor_copy(out=AIst[hi], in_=PWim[hi, :, 71]), r=[PWim], pw=[AIst])
                V(lambda e: e.tensor_scalar(out=AIsw[:], in0=AIst[:], scalar1=-1.0, scalar2=None, op0=ALU.mult), [AIst], [AIsw])
                fw.barrier()
            if STOP <= 1:
                return
            with ExitStack() as p1:
                Sst = sb(p1, "Sst", [128, 32, NSC], F32)
                Ssw = sb(p1, "Ssw", [128, 32, NSC], F32)
                F8 = sb(p1, "F8", [128, 8, 1024], BF16)
                f1 = sb(p1, "f1", [128, 4, 64, 16], F32)
                f2 = sb(p1, "f2", [128, 4, 64, 16], F32)
                Win = sb(p1, "Win", [128, 8, 8, 128], BF16)
                Wsw = sb(p1, "Wsw", [128, 8, 8, 128], BF16)
                U8 = sb(p1, "U8", [128, 8, NTH], BF16)
                for gs in range(4):
                    fw.dma("sp", U8[:], self.s_ublk[gs * 8:(gs + 1) * 8, :, :].rearrange("g p t -> p g t"), r=[self.s_ublk], w=[U8])
                    for q4 in range(2):
                        g0 = gs * 8 + q4 * 4
                        pr = PWre[:, g0:g0 + 4, 70:6:-1].unsqueeze(3).to_broadcast([128, 4, 64, 16])
                        pi = PWim[:, g0:g0 + 4, 70:6:-1].unsqueeze(3).to_broadcast([128, 4, 64, 16])
                        ba = BA[:, g0:g0 + 4, :].unsqueeze(2).to_broadcast([128, 4, 64, 16])
                        bb = BB[:, g0:g0 + 4, :].unsqueeze(2).to_broadcast([128, 4, 64, 16])
                        fw.op("dve", lambda e, pr=pr, ba=ba: e.tensor_tensor(out=f1[:], in0=pr, in1=ba, op=ALU.mult), r=[PWre, BA], w=[f1])
                        fw.op("pool", lambda e, pi=pi, bb=bb: e.tensor_tensor(out=f2[:], in0=pi, in1=bb, op=ALU.mult), r=[PWim, BB], w=[f2])
                        fw.op("dve", lambda e, q4=q4: e.tensor_tensor(out=F8[:, q4 * 4:(q4 + 1) * 4, :].rearrange("p g (j c) -> p g j c", c=16), in0=f1[:], in1=f2[:], op=ALU.add),
                              r=[f1, f2], pw=[F8])
                    fw.op("act", lambda e, gs=gs: e.activation(out=F7[:, gs * 8:(gs + 1) * 8, :], in_=F8[:, :, 56 * 16:64 * 16], func=AF.Copy), r=[F8], pw=[F7])
                    if STOP <= 2:
                        continue
                    for g in range(8):
                        pst = PS[g % 2]
                        pv = pst[:].bitcast(BF16)
                        for jh in range(8):
                            fw.op("pe", lambda e, g=g, jh=jh, pv=pv: e.transpose(pv[:, jh * 128:(jh + 1) * 128], F8[:, g, jh * 128:(jh + 1) * 128], self.ident[:]),
                                  r=[F8, self.ident], pw=[pst])
                        pv3 = pv.rearrange("p (j m) -> p j m", m=128)
                        fw.op("act", lambda e, g=g, pv3=pv3: e.activation(out=Win[:, g, :, :], in_=pv3, func=AF.Copy), r=[pst], pw=[Win])
                        if STOP == 3 and os.environ.get("SUB") == "a":
                            continue
                        fw.op("act", lambda e, g=g, pv3=pv3: e.activation(out=Wsw[:, g, :, 0:64], in_=pv3[:, :, 64:128], func=AF.Copy), r=[pst], pw=[Wsw])
                        fw.op("act", lambda e, g=g, pv3=pv3: e.activation(out=Wsw[:, g, :, 64:128], in_=pv3[:, :, 0:64], func=AF.Copy), r=[pst], pw=[Wsw])
                    if STOP <= 3:
                        continue
                    for (W_, S_, pb) in [(Win, Sst, PS[2]), (Wsw, Ssw, PS[3])]:
                        for g in range(8):
                            for jh in range(8):
                                fw.op("pe", lambda e, g=g, jh=jh, W_=W_, pb=pb: e.matmul(pb[:, g * NSC:(g + 1) * NSC], lhsT=W_[:, g, jh, :], rhs=U8[:, g, jh::8],
                                                                                      start=(jh == 0), stop=(jh == 7)), r=[W_, U8], pw=[pb])
                        fw.op("act", lambda e, S_=S_, pb=pb, gs=gs: e.activation(out=S_[:, gs * 8:(gs + 1) * 8, :], in_=pb[:, 0:8 * NSC].rearrange("p (g n) -> p g n", n=NSC), func=AF.Copy),
                              r=[pb], pw=[S_])
                if STOP <= 4:
                    fw.barrier()
                    return
                xa = [sb(p1, "xa%d" % i, [128, 32], F32) for i in range(2)]
                xb = [sb(p1, "xb%d" % i, [128, 32], F32) for i in range(2)]
                m1 = sb(p1, "m1", [128, 32], F32)
                m2 = sb(p1, "m2", [128, 32], F32)
                m3 = sb(p1, "m3", [128, 32], F32)
                m4 = sb(p1, "m4", [128, 32], F32)
                fw.op("dve", lambda e: e.memset(xa[0][:], 0.0), w=[xa[0]])
                fw.op("dve", lambda e: e.memset(xb[0][:], 0.0), w=[xb[0]])
                for n in range(NSC):
                    ca, cb_ = xa[n % 2], xb[n % 2]
                    na, nb = xa[(n + 1) % 2], xb[(n + 1) % 2]
                    fw.op("act", lambda e, n=n, ca=ca: e.activation(out=Xprev[:, :, n], in_=ca[:], func=AF.Copy), r=[ca], pw=[Xprev])
                    if n == NSC - 1:
                        break
                    fw.op("dve", lambda e, ca=ca: e.tensor_tensor(out=m1[:], in0=ca[:], in1=AR[:], op=ALU.mult), r=[ca, AR], w=[m1])
                    fw.op("dve", lambda e, cb_=cb_: e.tensor_tensor(out=m2[:], in0=cb_[:], in1=AIst[:], op=ALU.mult), r=[cb_, AIst], w=[m2])
                    fw.op("dve", lambda e, cb_=cb_: e.tensor_tensor(out=m3[:], in0=cb_[:], in1=AR[:], op=ALU.mult), r=[cb_, AR], w=[m3])
                    fw.op("dve", lambda e, ca=ca: e.tensor_tensor(out=m4[:], in0=ca[:], in1=AIsw[:], op=ALU.mult), r=[ca, AIsw], w=[m4])
                    fw.op("dve", lambda e: e.tensor_tensor(out=m1[:], in0=m1[:], in1=m2[:], op=ALU.add), r=[m1, m2], w=[m1])
                    fw.op("dve", lambda e: e.tensor_tensor(out=m3[:], in0=m3[:], in1=m4[:], op=ALU.add), r=[m3, m4], w=[m3])
                    fw.op("dve", lambda e, n=n, na=na: e.tensor_tensor(out=na[:], in0=m1[:], in1=Sst[:, :, n], op=ALU.add), r=[m1, Sst], w=[na])
                    fw.op("dve", lambda e, n=n, nb=nb: e.tensor_tensor(out=nb[:], in0=m3[:], in1=Ssw[:, :, n], op=ALU.add), r=[m3, Ssw], w=[nb])
                fw.barrier()
            if STOP <= 5:
                return
            with ExitStack() as p2:
                G8 = sb(p2, "G8", [128, 8, 72 * 16], BF16)
                g1 = sb(p2, "g1", [128, 2, 72, 16], F32)
                g2 = sb(p2, "g2", [128, 2, 72, 16], F32)
                Wi = sb(p2, "Wi", [128, 8, 1024], BF16)
                U8 = sb(p2, "U8b", [128, 8, NTH], BF16)
                Yact = sb(p2, "Yact", [128, 8, NTH], BF16)
                Y8 = sb(p2, "Y8", [128, NT8, 8, 512], BF16)
                ysT = sb(p2, "ysT", [128, 4, T], BF16)
                ylin = [sb(p2, "ylin%d" % i, [128, NTH], F32) for i in range(2)]
                gw = sb(p2, "gw", [128, 4, 512], BF16)
                gb = sb(p2, "gb", [128, 4], F32)
                fw.dma("pool", gw[:], self.glu_w[l].rearrange("(kc p) m -> p kc m", p=128), w=[gw])
                fw.dma("sp", gb[:], self.glu_b[l], w=[gb])
                for gs in range(4):
                    fw.dma("sp", U8[:], self.s_ublk[gs * 8:(gs + 1) * 8, :, :].rearrange("g p t -> p g t"), r=[self.s_ublk], w=[U8])
                    for q2 in range(4):
                        g0 = gs * 8 + q2 * 2
                        pr = PWre[:, g0:g0 + 2, :].unsqueeze(3).to_broadcast([128, 2, 72, 16])
                        pi = PWim[:, g0:g0 + 2, :].unsqueeze(3).to_broadcast([128, 2, 72, 16])
                        ca = CA[:, g0:g0 + 2, :].unsqueeze(2).to_broadcast([128, 2, 72, 16])
                        cb2 = CB[:, g0:g0 + 2, :].unsqueeze(2).to_broadcast([128, 2, 72, 16])
                        fw.op("dve", lambda e, pr=pr, ca=ca: e.tensor_tensor(out=g1[:], in0=pr, in1=ca, op=ALU.mult), r=[PWre, CA], w=[g1])
                        fw.op("pool", lambda e, pi=pi, cb2=cb2: e.tensor_tensor(out=g2[:], in0=pi, in1=cb2, op=ALU.mult), r=[PWim, CB], w=[g2])
                        fw.op("dve", lambda e, q2=q2: e.tensor_tensor(out=G8[:, q2 * 2:(q2 + 1) * 2, :].rearrange("p g (j c) -> p g j c", c=16), in0=g1[:], in1=g2[:], op=ALU.add),
                              r=[g1, g2], pw=[G8])
                    if STOP <= 6:
                        continue
                    for g in range(8):
                        gi = gs * 8 + g
                        pa, pb = PS[0 + 2 * (g % 2)], PS[1 + 2 * (g % 2)]
                        fw.op("pe", lambda e, g=g, gi=gi, pa=pa: e.matmul(pa[:], lhsT=F7[:, gi, :], rhs=G8[:, g, 0:512], start=True, stop=True), r=[F7, G8], w=[pa])
                        fw.op("pe", lambda e, g=g, gi=gi, pb=pb: e.matmul(pb[:], lhsT=F7[:, gi, :], rhs=G8[:, g, 512:1024], start=True, stop=True), r=[F7, G8], w=[pb])
                        fw.op("dve", lambda e, g=g, pa=pa: e.tensor_tensor(out=Wi[:, g, 0:128], in0=pa[:, 0:128], in1=mask8[:], op=ALU.mult), r=[pa, mask8], pw=[Wi])
                        fw.op("act", lambda e, g=g, pa=pa: e.activation(out=Wi[:, g, 128:512], in_=pa[:, 128:512], func=AF.Copy), r=[pa], pw=[Wi])
                        fw.op("act", lambda e, g=g, pb=pb: e.activation(out=Wi[:, g, 512:1024], in_=pb[:], func=AF.Copy), r=[pb], pw=[Wi])
                    if STOP <= 7:
                        continue
                    for g in range(8):
                        gi = gs * 8 + g
                        yps = PS[4 + (g % 2)]
                        y3 = yps[:, 0:NTH].rearrange("p (n i) -> p n i", i=8)
                        u3 = U8[:, g, :].rearrange("p (n i) -> p n i", i=8)
                        for d in range(8):
                            fw.op("pe", lambda e, g=g, d=d, y3=y3, u3=u3: e.matmul(y3[:, :, d:8], lhsT=Wi[:, g, d * 128:(d + 1) * 128], rhs=u3[:, :, 0:8 - d],
                                                                                 start=(d == 0), stop=False), r=[Wi, U8], pw=[yps])
                        for ih in range(8):
                            fw.op("pe", lambda e, g=g, gi=gi, ih=ih, y3=y3: e.matmul(y3[:, :, ih], lhsT=G8[:, g, (ih + 1) * 128:(ih + 2) * 128], rhs=Xprev[:, gi, :],
                                                                                   start=False, stop=(ih == 7)), r=[G8, Xprev], pw=[yps])
                        yl = ylin[g % 2]
                        fw.op("dve", lambda e, g=g, gi=gi, yl=yl, yps=yps: e.scalar_tensor_tensor(out=yl[:], in0=U8[:, g, :], scalar=dblk[:, gi:gi + 1], in1=yps[:, 0:NTH],
                                                                                               op0=ALU.mult, op1=ALU.add), r=[U8, dblk, yps], w=[yl])
                        fw.op("act", lambda e, g=g, yl=yl: e.activation(out=Yact[:, g, :], in_=yl[:], func=AF.Gelu), r=[yl], pw=[Yact])
                    if STOP <= 8:
                        continue
                    for thi in range(NT8):
                        pst = PS[6 + (thi % 2)]
                        pv = pst[:].bitcast(BF16)
                        for g in range(8):
                            fw.op("pe", lambda e, g=g, thi=thi, pv=pv: e.transpose(pv[:, g * 128:(g + 1) * 128], Yact[:, g, thi * 128:(thi + 1) * 128], self.ident[:]),
                                  r=[Yact, self.ident], pw=[pst])
                        fw.op("act", lambda e, thi=thi, pv=pv, gs=gs: e.activation(out=Y8[:, thi, :, gs * 128:(gs + 1) * 128].rearrange("p i (g c) -> p i g c", c=16),
                                                                             in_=pv.rearrange("p (g i c) -> p i g c", i=8, c=16), func=AF.Copy), r=[pst], pw=[Y8])
                if STOP <= 9:
                    fw.barrier()
                    return
                k = 0
                for thi in range(NT8):
                    for kc in range(4):
                        pst = PS[6 + (k % 2)]
                        k += 1
                        pv = pst[:].bitcast(BF16)
                        for il in range(8):
                            fw.op("pe", lambda e, il=il, thi=thi, kc=kc, pv=pv: e.transpose(pv[:, il * 128:(il + 1) * 128], Y8[:, thi, il, kc * 128:(kc + 1) * 128], self.ident[:]),
                                  r=[Y8, self.ident], pw=[pst])
                        eng = "act"
                        if eng == "dve":
                            fw.op("dve", lambda e, thi=thi, kc=kc, pv=pv: e.tensor_copy(out=ysT[:, kc, thi * 1024:(thi + 1) * 1024].rearrange("p (t i) -> p t i", i=8),
                                                                                 in_=pv.rearrange("p (i t) -> p t i", t=128)), r=[pst], pw=[ysT])
                        else:
                            fw.op("act", lambda e, thi=thi, kc=kc, pv=pv: e.activation(out=ysT[:, kc, thi * 1024:(thi + 1) * 1024].rearrange("p (t i) -> p t i", i=8),
                                                                                in_=pv.rearrange("p (i t) -> p t i", t=128), func=AF.Copy), r=[pst], pw=[ysT])
                stg = [sb(p2, "gstg%d" % i, [128, ST], BF16) for i in range(3)]
                gsg = [sb(p2, "gsg%d" % i, [128, ST], BF16) for i in range(2)]
                k = 0
                for st in range(self.NST):
                    t0 = st * ST
                    for jc in range(4):
                        ps = PS[k % 4]
                        for kc in range(4):
                            fw.op("pe", lambda e, kc=kc, jc=jc, ps=ps, t0=t0: e.matmul(ps[:], lhsT=gw[:, kc, jc * 128:(jc + 1) * 128], rhs=ysT[:, kc, t0:t0 + ST],
                                                                                   start=(kc == 0), stop=(kc == 3)), r=[gw, ysT], pw=[ps])
                        sg = gsg[k % 2]
                        so = stg[k % 3]
                        k += 1
                        fw.op("act", lambda e, sg=sg, ps=ps, jc=jc: e.activation(out=sg[:], in_=ps[:], func=AF.Sigmoid, bias=gb[:, jc:jc + 1]), r=[ps, gb], w=[sg])
                        fw.op("dve", lambda e, sg=sg, so=so, jc=jc, t0=t0: e.tensor_tensor(out=so[:], in0=sg[:], in1=ysT[:, jc, t0:t0 + ST], op=ALU.mult), r=[sg, ysT], w=[so])
                        fw.dma("pool", self.s_ys5[jc * 128:(jc + 1) * 128, t0:t0 + ST], so[:], r=[so], pw=[self.s_ys5])
                fw.barrier()

    def phase_B1(self, l):
        nc, fw, sb = self.nc, self.fw, self.sb
        T, NCH = self.T, self.NCH
        with ExitStack() as pes:
            A = sb(pes, "b1A", [4, T], F32)
            B = sb(pes, "b1B", [4, T], F32)
            C = sb(pes, "b1C", [4, T], F32)
            M = sb(pes, "b1M", [4, T], F32)
            ifb = sb(pes, "ifb", [4, 2], F32)
            nfb = sb(pes, "nfb", [4, 1], F32)
            umax = sb(pes, "umax", [4, NCH], F32)
            blast = sb(pes, "blast", [4, NCH], F32)
            mnext = sb(pes, "mnext", [4, NCH], F32)
            mprev = sb(pes, "mprev", [4, NCH], F32)
            Rr = sb(pes, "Rr", [4, NCH], F32)
            fw.dma("sp", A[:], self.s_mi[:, :], r=[self.s_mi], w=[A])
            fw.dma("sp", B[:], self.s_mf[:, :], r=[self.s_mf], w=[B])
            fw.dma("sp", ifb[:], self.ml_ifb[l], w=[ifb])
            V = lambda fn, r, w: fw.op("dve", fn, r=r, w=w)
            V(lambda e: e.tensor_scalar(out=nfb[:], in0=ifb[:, 1:2], scalar1=-1.0, scalar2=None, op0=ALU.mult), [ifb], [nfb])
            V(lambda e: e.memset(M[:], 1.0), [], [M])
            V(lambda e: e.memset(M[:].rearrange("p (c t) -> p c t", t=CH)[:, :, 0:1], 0.0), [M], [M])
            V(lambda e: e.tensor_scalar(out=A[:], in0=A[:], scalar1=ifb[:, 0:1], scalar2=None, op0=ALU.add), [A, ifb], [A])
            fw.op("act", lambda e: e.activation(out=B[:], in_=B[:], func=AF.Exp, scale=-1.0, bias=nfb[:, 0:1]), r=[B, nfb], w=[B])
            fw.op("act", lambda e: e.activation(out=B[:], in_=B[:], func=AF.Ln, bias=1.0), r=[B], w=[B])
            V(lambda e: e.tensor_tensor_scan(out=C[:], data0=M[:], data1=B[:], initial=0.0, op0=ALU.mult, op1=ALU.add), [M, B], [C])
            V(lambda e: e.tensor_tensor(out=A[:], in0=A[:], in1=C[:], op=ALU.add), [A, C], [A])
            V(lambda e: e.tensor_reduce(out=umax[:], in_=A[:].rearrange("p (c t) -> p c t", t=CH), axis=AX.X, op=ALU.max), [A], [umax])
            V(lambda e: e.tensor_scalar(out=blast[:], in0=C[:, CH - 1::CH], scalar1=-1.0, scalar2=None, op0=ALU.mult), [C], [blast])
            V(lambda e: e.tensor_tensor_scan(out=mnext[:], data0=umax[:], data1=blast[:], initial=0.0, op0=ALU.max, op1=ALU.add), [umax, blast], [mnext])
            V(lambda e: e.tensor_tensor(out=Rr[:], in0=mnext[:], in1=blast[:], op=ALU.subtract), [mnext, blast], [Rr])
            V(lambda e: e.memset(mprev[:], 0.0), [], [mprev])
            if NCH > 1:
                V(lambda e: e.tensor_copy(out=mprev[:, 1:NCH], in_=mnext[:, 0:NCH - 1]), [mnext, mprev], [mprev])
            V(lambda e: e.tensor_tensor(out=mprev[:], in0=mprev[:], in1=Rr[:], op=ALU.subtract), [mprev, Rr], [mprev])
            fw.op("act", lambda e: e.activation(out=self.decay[:], in_=mprev[:], func=AF.Exp), r=[mprev], w=[self.decay])
            R3 = Rr[:, :].unsqueeze(2).to_broadcast([4, NCH, CH])
            V(lambda e: e.tensor_tensor(out=A[:].rearrange("p (c t) -> p c t", t=CH), in0=A[:].rearrange("p (c t) -> p c t", t=CH), in1=R3, op=ALU.subtract), [A, Rr], [A])
            fw.op("act", lambda e: e.activation(out=A[:], in_=A[:], func=AF.Exp, bias=self.ln8[0:4, 0:1]), r=[A, self.ln8], w=[A])
            V(lambda e: e.tensor_tensor(out=C[:].rearrange("p (c t) -> p c t", t=CH), in0=C[:].rearrange("p (c t) -> p c t", t=CH), in1=R3, op=ALU.subtract), [C, Rr], [C])
            fw.op("act", lambda e: e.activation(out=C[:], in_=C[:], func=AF.Exp), r=[C], w=[C])
            fw.dma("sp", self.s_kfac[:, :], A[:], r=[A], w=[self.s_kfac])
            fw.dma("sp", self.s_floor[:, :], C[:], r=[C], w=[self.s_floor])
            fw.barrier()

    def phase_B2(self, l):
        nc, fw, sb = self.nc, self.fw, self.sb
        PS = self.PS
        T, NCH = self.T, self.NCH
        with ExitStack() as pes:
            pj = [sb(pes, "pj%d" % i, [128, 4, D], BF16) for i in range(3)]
            wo = sb(pes, "wo", [128, 8, D], BF16)
            for i in range(3):
                fw.dma("pool", pj[i][:], self.proj[i][l].rearrange("(kc p) m -> p kc m", p=128), w=[pj[i]])
            for kc in range(8):
                fw.dma("pool", wo[:, kc, :], self.w_out[l, kc * 128:(kc + 1) * 128, :], pw=[wo])
            gnw = sb(pes, "gnw", [128, 4], F32)
            cw = sb(pes, "cw", [128, 4, 4], F32)
            cb = sb(pes, "cb", [128, 4], F32)
            fw.dma("sp", gnw[:], self.gla_nw[l], w=[gnw])
            fw.dma("sp", cw[:], self.conv_w[l], w=[cw])
            fw.dma("sp", cb[:], self.conv_b[l], w=[cb])
            decb = sb(pes, "decb", [128, 2, NCH], F32)
            for hp in range(2):
                fw.op("pe", lambda e, hp=hp: e.matmul(PS[7][:, 0:NCH], lhsT=self.selk[hp][:], rhs=self.decay[:], start=True, stop=True), r=[self.selk[hp], self.decay], w=[PS[7]])
                fw.op("dve", lambda e, hp=hp: e.tensor_copy(out=decb[:, hp, :], in_=PS[7][:, 0:NCH]), r=[PS[7]], pw=[decb])
            gq = sb(pes, "gq", [128, 2, ST], BF16)
            gk = sb(pes, "gk", [128, 2, ST], BF16)
            gg = sb(pes, "gg", [128, 2, ST], F32)
            gr = sb(pes, "gr", [128, 4, ST], BF16)
            gv = sb(pes, "gv", [128, 4, 512], BF16)
            mqk = sb(pes, "mqk", [128, 4, ST + 3], BF16)
            mo = sb(pes, "mo", [128, 4, ST], BF16)
            mv = sb(pes, "mv", [128, 4, 512], BF16)
            kfac = sb(pes, "kfac", [4, ST], F32)
            flo = sb(pes, "flo", [4, ST], F32)
            gts = [sb(pes, "gates%d" % i, [128, 3, ST], BF16) for i in range(2)]
            ys5 = sb(pes, "ys5", [128, 4, ST], BF16)
            xs = sb(pes, "xsb", [128, 8, ST], F32)
            cum = sb(pes, "cum", [128, ST], F32)
            eq = sb(pes, "eq", [128, ST], F32)
            ek = sb(pes, "ek", [128, ST], F32)
            ekh = sb(pes, "ekh", [128, ST], F32)
            ecl = sb(pes, "ecl", [128, 2, 4], F32)
            qt = sb(pes, "qt", [128, 2, ST], BF16)
            kt = sb(pes, "kt", [128, 2, ST], BF16)
            kh = sb(pes, "kh", [128, 2, ST], BF16)
            khTM = sb(pes, "khTM", [128, 4, 256], BF16)
            ATs = [sb(pes, "AT%d" % i, [128, 128], BF16) for i in range(3)]
            S = sb(pes, "S", [128, 2, 128], F32)
            Sbf = sb(pes, "Sbf", [128, 2, 128], BF16)
            osq = sb(pes, "osq", [128, ST], BF16)
            rs = sb(pes, "rs", [128, ST], F32)
            tmp = sb(pes, "tmpb", [128, ST], F32)
            ygla = sb(pes, "ygla", [128, 4, ST], BF16)
            yml = sb(pes, "yml", [128, 4, ST], BF16)
            acc = sb(pes, "acc", [128, ST], F32)
            mqs = sb(pes, "mqs", [128, 2, ST], BF16)
            mks = sb(pes, "mks", [128, ST], F32)
            ktl = sb(pes, "ktl", [128, 2, ST], BF16)
            ktlTM = sb(pes, "ktlTM", [128, 4, 256], BF16)
            ksum = sb(pes, "ksum", [128, 2, 4], F32)
            mem = sb(pes, "mem", [128, 2, 128], F32)
            memS = sb(pes, "memS", [128, 2, 128], F32)
            memSbf = sb(pes, "memSbf", [128, 2, 128], BF16)
            nrm = sb(pes, "nrm", [128, 2], F32)
            nrmS = sb(pes, "nrmS", [128, 2], F32)
            nbc = sb(pes, "nbc", [128, 2, 128], BF16)
            fls = sb(pes, "fls", [128, ST], F32)
            mrg = sb(pes, "mrg", [128, ST], F32)
            mtmp = [sb(pes, "mtmp%d" % i, [128, ST], F32) for i in range(2)]
            mT = sb(pes, "mT", [128, 8, ST], BF16)
            ysb = sb(pes, "ysb", [128, 8, ST], F32)
            sq = sb(pes, "sqb", [128, 8, ST], BF16)
            rstd = sb(pes, "rstdb", [128, ST], F32)
            for t_ in (S, Sbf, mem, nrm):
                fw.op("dve", lambda e, t_=t_: e.memset(t_[:], 0.0), w=[t_])
            cntr = {"at": 0, "ps": 0}
            ATV = [TT(PS[6].t, "atv0"), TT(PS[6].t, "atv1")]
            STV = TT(PS[6].t, "stv")
            tmp2 = sb(pes, "tmp2b", [128, ST], F32)

            def chunk_attention(c, h, kT_t, qT_t, v_tm, st_bf, o_ps, den=None):
                hp, off = h // 2, 64 * (h % 2)
                cs = slice(c * CH, (c + 1) * CH)
                cntr["at"] += 1
                aps = PS[6]
                AT = ATs[cntr["at"] % 3]
                fw.op("pe", lambda e: e.matmul(aps[:, 0:CH], lhsT=kT_t[off:off + 64, hp, cs], rhs=qT_t[off:off + 64, hp, cs], start=True, stop=True),
                      r=[kT_t, qT_t], w=[aps])
                fw.op("dve", lambda e: e.tensor_tensor(out=AT[:], in0=aps[:, 0:CH], in1=self.maskT[:], op=ALU.mult), r=[aps, self.maskT], w=[AT])
                fw.op("pe", lambda e: e.matmul(o_ps[:, cs], lhsT=v_tm[:, c, h * 128:(h + 1) * 128], rhs=AT[:], start=True, stop=False), r=[v_tm, AT], pw=[o_ps])
                fw.op("pe", lambda e: e.matmul(o_ps[:, cs], lhsT=st_bf[off:off + 64, hp, :], rhs=qT_t[off:off + 64, hp, cs], start=False, stop=True),
                      r=[st_bf, qT_t], pw=[o_ps])
                if den is not None:
                    d_ps, nb_ = den
                    fw.op("pe", lambda e: e.matmul(d_ps[:, cs], lhsT=self.ones_bf[:], rhs=AT[:], start=True, stop=False), r=[self.ones_bf, AT], pw=[d_ps])
                    fw.op("pe", lambda e: e.matmul(d_ps[:, cs], lhsT=nb_[off:off + 64, hp, :], rhs=qT_t[off:off + 64, hp, cs], start=False, stop=True),
                          r=[nb_, qT_t], pw=[d_ps])

            def issue_chain_loads(st):
                t0 = st * ST
                if True:
                    fw.dma("sp", gq[:], self.s_gq[:, t0:t0 + ST].rearrange("(a p) t -> p a t", p=128), r=[self.s_gq], w=[gq])
                    fw.dma("sp", gk[:], self.s_gk[:, t0:t0 + ST].rearrange("(a p) t -> p a t", p=128), r=[self.s_gk], w=[gk])
                    fw.dma("sp", gg[:], self.s_g[:, t0:t0 + ST].rearrange("(a p) t -> p a t", p=128), r=[self.s_g], w=[gg])
                    fw.dma("sp", gr[:], self.s_gr[:, t0:t0 + ST].rearrange("(a p) t -> p a t", p=128), r=[self.s_gr], w=[gr])
                    fw.dma("sp", gv[:], self.s_gv[t0:t0 + ST, :].rearrange("(a p) m -> p a m", p=128), r=[self.s_gv], w=[gv])
                    if st == 0:
                        fw.op("dve", lambda e: e.memset(mqk[:, :, 0:3], 0.0), w=[mqk])
                        fw.dma("sp", mqk[:, :, 3:ST + 3], self.s_mqk[:, 0:ST].rearrange("(a p) t -> p a t", p=128), r=[self.s_mqk], pw=[mqk])
                    else:
                        fw.dma("sp", mqk[:], self.s_mqk[:, t0 - 3:t0 + ST].rearrange("(a p) t -> p a t", p=128), r=[self.s_mqk], w=[mqk])
                    fw.dma("sp", mo[:], self.s_mo[:, t0:t0 + ST].rearrange("(a p) t -> p a t", p=128), r=[self.s_mo], w=[mo])
                    fw.dma("sp", mv[:], self.s_mv[t0:t0 + ST, :].rearrange("(a p) m -> p a m", p=128), r=[self.s_mv], w=[mv])
                    fw.dma("sp", kfac[:], self.s_kfac[:, t0:t0 + ST], r=[self.s_kfac], w=[kfac])
                    fw.dma("sp", flo[:], self.s_floor[:, t0:t0 + ST], r=[self.s_floor], w=[flo])

            lo, hi = slice(0, 64), slice(64, 128)
            import os
            BSTOP = float(os.environ.get("B2STOP", "99"))
            for st in range(self.NST):
                t0 = st * ST
                L_ = lambda dst, src, rr: fw.dma("sp", dst, src, r=[rr], w=[])
                if st == 0:
                    issue_chain_loads(0)
                fw.dma("sp", ys5[:], self.s_ys5[:, t0:t0 + ST].rearrange("(a p) t -> p a t", p=128), r=[self.s_ys5], w=[ys5])
                fw.dma("sp", xs[:], self.xT[:, t0:t0 + ST].rearrange("(kc p) t -> p kc t", p=128), r=[self.xT], w=[xs])
                for hp in range(2):
                    fw.op("dve", lambda e, hp=hp: e.tensor_tensor_scan(out=cum[:], data0=self.mask01[:], data1=gg[:, hp, :], initial=0.0, op0=ALU.mult, op1=ALU.add),
                          r=[self.mask01, gg], w=[cum])
                    fw.op("act", lambda e: e.activation(out=eq[:], in_=cum[:], func=AF.Exp), r=[cum], w=[eq])
                    fw.op("dve", lambda e, hp=hp: e.scalar_tensor_tensor(out=qt[:, hp, :], in0=gq[:, hp, :], scalar=0.125, in1=eq[:], op0=ALU.mult, op1=ALU.mult),
                          r=[gq, eq], pw=[qt])
                    fw.op("act", lambda e: e.activation(out=ek[:], in_=cum[:], func=AF.Exp, scale=-1.0), r=[cum], w=[ek])
                    fw.op("dve", lambda e, hp=hp: e.tensor_tensor(out=kt[:, hp, :], in0=gk[:, hp, :], in1=ek[:], op=ALU.mult), r=[gk, ek], pw=[kt])
                    for c in range(4):
                        fw.op("act", lambda e, c=c: e.activation(out=ekh[:, c * CH:(c + 1) * CH], in_=cum[:, c * CH:(c + 1) * CH], func=AF.Exp, scale=-1.0,
                                                                 bias=cum[:, c * CH + CH - 1:c * CH + CH]), r=[cum], pw=[ekh])
                    fw.op("dve", lambda e, hp=hp: e.tensor_tensor(out=kh[:, hp, :], in0=gk[:, hp, :], in1=ekh[:], op=ALU.mult), r=[gk, ekh], pw=[kh])
                    fw.op("act", lambda e, hp=hp: e.activation(out=ecl[:, hp, :], in_=cum[:, CH - 1::CH], func=AF.Exp), r=[cum], pw=[ecl])
                    pv = PS[7][:].bitcast(BF16)
                    for c in range(4):
                        fw.op("pe", lambda e, c=c, hp=hp, pv=pv: e.transpose(pv[:, c * 128:(c + 1) * 128], kh[:, hp, c * CH:(c + 1) * CH], self.ident[:]),
                              r=[kh, self.ident], pw=[PS[7]])
                    fw.op("act", lambda e, hp=hp, pv=pv: e.activation(out=khTM[:, :, hp * 128:(hp + 1) * 128], in_=pv[:, 0:512].rearrange("p (c m) -> p c m", m=128), func=AF.Copy),
                          r=[PS[7]], pw=[khTM])
                for ti in range(4):
                    fw.op("dve", lambda e, ti=ti: e.tensor_scalar(out=acc[:], in0=mqk[:, ti, 3:ST + 3], scalar1=cw[:, ti, 3:4], scalar2=cb[:, ti:ti + 1], op0=ALU.mult, op1=ALU.add),
                          r=[mqk, cw, cb], w=[acc])
                    for k in (2, 1, 0):
                        fw.op("dve", lambda e, ti=ti, k=k: e.scalar_tensor_tensor(out=acc[:], in0=mqk[:, ti, k:ST + k], scalar=cw[:, ti, k:k + 1], in1=acc[:], op0=ALU.mult, op1=ALU.add),
                              r=[mqk, cw, acc], w=[acc])
                    if ti < 2:
                        fw.op("act", lambda e, ti=ti: e.activation(out=mqs[:, ti, :], in_=acc[:], func=AF.Silu), r=[acc], pw=[mqs])
                    else:
                        hp = ti - 2
                        fw.op("act", lambda e: e.activation(out=mks[:], in_=acc[:], func=AF.Silu), r=[acc], w=[mks])
                        fw.op("pe", lambda e, hp=hp: e.matmul(PS[7][:], lhsT=self.selk[hp][:], rhs=kfac[:], start=True, stop=True), r=[self.selk[hp], kfac], w=[PS[7]])
                        fw.op("dve", lambda e, hp=hp: e.tensor_tensor(out=ktl[:, hp, :], in0=mks[:], in1=PS[7][:], op=ALU.mult), r=[mks, PS[7]], pw=[ktl])
                        fw.op("dve", lambda e, hp=hp: e.tensor_reduce(out=ksum[:, hp, :], in_=ktl[:, hp, :].rearrange("p (c t) -> p c t", t=CH), axis=AX.X, op=ALU.add),
                              r=[ktl], pw=[ksum])
                        pv = PS[7][:].bitcast(BF16)
                        for c in range(4):
                            fw.op("pe", lambda e, c=c, hp=hp, pv=pv: e.transpose(pv[:, c * 128:(c + 1) * 128], ktl[:, hp, c * CH:(c + 1) * CH], self.ident[:]),
                                  r=[ktl, self.ident], pw=[PS[7]])
                        fw.op("act", lambda e, hp=hp, pv=pv: e.activation(out=ktlTM[:, :, hp * 128:(hp + 1) * 128], in_=pv[:, 0:512].rearrange("p (c m) -> p c m", m=128), func=AF.Copy),
                              r=[PS[7]], pw=[ktlTM])
                for hp in range(2):
                    gla_o = [PS[0], PS[1]]
                    ml_o = [PS[2], PS[3]]
                    ml_d = [PS[4], PS[5]]
                    for c in range(4):
                        cg = st * 4 + c
                        for hh in range(2):
                            h = hp * 2 + hh
                            chunk_attention(c, h, kt, qt, gv, Sbf, gla_o[hh])
                            fw.op("pe", lambda e, c=c, h=h, hh=hh, hp=hp: e.matmul(PS[7][:, hh * 128:(hh + 1) * 128], lhsT=khTM[:, c, hp * 128:(hp + 1) * 128],
                                                                                 rhs=gv[:, c, h * 128:(h + 1) * 128], start=True, stop=True), r=[khTM, gv], pw=[PS[7]])
                        fw.op("dve", lambda e, c=c, hp=hp: e.scalar_tensor_tensor(out=S[lo, hp, :], in0=S[lo, hp, :], scalar=ecl[lo, hp, c:c + 1], in1=PS[7][lo, 0:128],
                                                                                op0=ALU.mult, op1=ALU.add), r=[S, ecl, PS[7]], pw=[S])
                        fw.op("dve", lambda e, c=c, hp=hp: e.scalar_tensor_tensor(out=S[hi, hp, :], in0=S[hi, hp, :], scalar=ecl[hi, hp, c:c + 1], in1=PS[7][hi, 128:256],
                                                                                op0=ALU.mult, op1=ALU.add), r=[S, ecl, PS[7]], pw=[S])
                        fw.op("act", lambda e, hp=hp: e.activation(out=Sbf[:, hp, :], in_=S[:, hp, :], func=AF.Copy), r=[S], pw=[Sbf])
                        fw.op("dve", lambda e, hp=hp, cg=cg: e.tensor_scalar(out=memS[:, hp, :], in0=mem[:, hp, :], scalar1=decb[:, hp, cg:cg + 1], scalar2=None, op0=ALU.mult),
                              r=[mem, decb], pw=[memS])
                        fw.op("act", lambda e, hp=hp: e.activation(out=memSbf[:, hp, :], in_=memS[:, hp, :], func=AF.Copy), r=[memS], pw=[memSbf])
                        fw.op("dve", lambda e, hp=hp, cg=cg: e.tensor_scalar(out=nrmS[:, hp:hp + 1], in0=nrm[:, hp:hp + 1], scalar1=decb[:, hp, cg:cg + 1], scalar2=None, op0=ALU.mult),
                              r=[nrm, decb], pw=[nrmS])
                        fw.op("dve", lambda e, hp=hp: e.tensor_copy(out=nbc[:, hp, :], in_=nrmS[:, hp:hp + 1].to_broadcast([128, 128])), r=[nrmS], pw=[nbc])
                        for hh in range(2):
                            h = hp * 2 + hh
                            chunk_attention(c, h, ktl, mqs, mv, memSbf, ml_o[hh], den=(ml_d[hh], nbc))
                            fw.op("pe", lambda e, c=c, h=h, hh=hh, hp=hp: e.matmul(PS[7][:, hh * 128:(hh + 1) * 128], lhsT=ktlTM[:, c, hp * 128:(hp + 1) * 128],
                                                                                 rhs=mv[:, c, h * 128:(h + 1) * 128], start=True, stop=True), r=[ktlTM, mv], pw=[PS[7]])
                        fw.op("dve", lambda e, hp=hp: e.tensor_tensor(out=mem[lo, hp, :], in0=memS[lo, hp, :], in1=PS[7][lo, 0:128], op=ALU.add), r=[memS, PS[7]], pw=[mem])
                        fw.op("dve", lambda e, hp=hp: e.tensor_tensor(out=mem[hi, hp, :], in0=memS[hi, hp, :], in1=PS[7][hi, 128:256], op=ALU.add), r=[memS, PS[7]], pw=[mem])
                        fw.op("dve", lambda e, hp=hp, c=c: e.tensor_tensor(out=nrm[:, hp:hp + 1], in0=nrmS[:, hp:hp + 1], in1=ksum[:, hp, c:c + 1], op=ALU.add),
                              r=[nrmS, ksum], pw=[nrm])
                    for hh in range(2):
                        h = hp * 2 + hh
                        op_ = gla_o[hh]
                        fw.op("act", lambda e, op_=op_: e.activation(out=osq[:], in_=op_[:], func=AF.Square), r=[op_], w=[osq])
                        fw.op("pe", lambda e: e.matmul(PS[6][:], lhsT=self.ones_bf[:], rhs=osq[:], start=True, stop=True), r=[self.ones_bf, osq], w=[PS[6]])
                        fw.op("act", lambda e: e.activation(out=rs[:], in_=PS[6][:], func=AF.Ln, scale=1.0 / 128.0, bias=self.eps_t[:, 0:1]), r=[PS[6], self.eps_t], w=[rs])
                        fw.op("act", lambda e: e.activation(out=rs[:], in_=rs[:], func=AF.Exp, scale=-0.5), r=[rs], w=[rs])
                        fw.op("dve", lambda e, op_=op_, h=h: e.scalar_tensor_tensor(out=tmp[:], in0=op_[:], scalar=gnw[:, h:h + 1], in1=rs[:], op0=ALU.mult, op1=ALU.mult),
                              r=[op_, gnw, rs], w=[tmp])
                        fw.op("dve", lambda e, h=h: e.tensor_tensor(out=ygla[:, h, :], in0=tmp[:], in1=gr[:, h, :], op=ALU.mult), r=[tmp, gr], pw=[ygla])
                    for hh in range(2):
                        h = hp * 2 + hh
                        op_, dp_ = ml_o[hh], ml_d[hh]
                        fw.op("pe", lambda e, h=h: e.matmul(PS[6][:], lhsT=self.selv[h][:], rhs=flo[:], start=True, stop=True), r=[self.selv[h], flo], w=[PS[6]])
                        fw.op("act", lambda e: e.activation(out=fls[:], in_=PS[6][:], func=AF.Copy), r=[PS[6]], w=[fls])
                        fw.op("dve", lambda e, dp_=dp_: e.tensor_tensor(out=tmp2[:], in0=dp_[:], in1=fls[:], op=ALU.max), r=[dp_, fls], w=[tmp2])
                        fw.op("dve", lambda e, dp_=dp_: e.scalar_tensor_tensor(out=tmp2[:], in0=dp_[:], scalar=-1.0, in1=tmp2[:], op0=ALU.mult, op1=ALU.max), r=[dp_, tmp2], w=[tmp2])
                        fw.op("act", lambda e: e.activation(out=tmp2[:], in_=tmp2[:], func=AF.Ln), r=[tmp2], w=[tmp2])
                        fw.op("act", lambda e: e.activation(out=tmp2[:], in_=tmp2[:], func=AF.Exp, scale=-1.0), r=[tmp2], w=[tmp2])
                        fw.op("dve", lambda e, op_=op_: e.tensor_tensor(out=tmp2[:], in0=op_[:], in1=tmp2[:], op=ALU.mult), r=[op_, tmp2], w=[tmp2])
                        fw.op("dve", lambda e, h=h: e.tensor_tensor(out=yml[:, h, :], in0=tmp2[:], in1=mo[:, h, :], op=ALU.mult), r=[tmp2, mo], pw=[yml])
                if st + 1 < self.NST:
                    issue_chain_loads(st + 1)
                if BSTOP <= 5:
                    continue
                if self.debug:
                    fw.dma("pool", self.s_ygla[:, t0:t0 + ST].rearrange("(a p) t -> p a t", p=128), ygla[:], r=[ygla], pw=[self.s_ygla])
                    fw.dma("pool", self.s_yml[:, t0:t0 + ST].rearrange("(a p) t -> p a t", p=128), yml[:], r=[yml], pw=[self.s_yml])
                k = 0
                for j in range(8):
                    gates = gts[j % 2]
                    fw.dma("sp", gates[:], self.s_gates[:, t0:t0 + ST].rearrange("(b j p) t -> j p b t", b=3, p=128)[j], r=[self.s_gates], w=[gates])
                    for bi, yT in enumerate((ygla, yml, ys5)):
                        ps = PS[k % 4]
                        k += 1
                        for kc in range(4):
                            fw.op("pe", lambda e, kc=kc, j=j, bi=bi, yT=yT, ps=ps: e.matmul(ps[:], lhsT=pj[bi][:, kc, j * 128:(j + 1) * 128], rhs=yT[:, kc, :],
                                                                                          start=(kc == 0), stop=(kc == 3)), r=[pj[bi], yT], pw=[ps])
                        if bi == 0:
                            fw.op("dve", lambda e, j=j, ps=ps, gates=gates: e.tensor_tensor(out=mrg[:], in0=gates[:, 0, :], in1=ps[:], op=ALU.mult), r=[gates, ps], w=[mrg])
                        else:
                            mt = mtmp[bi - 1]
                            fw.op("dve", lambda e, j=j, ps=ps, bi=bi, mt=mt, gates=gates: e.tensor_tensor(out=mt[:], in0=gates[:, bi, :], in1=ps[:], op=ALU.mult), r=[gates, ps], w=[mt])
                            if bi == 1:
                                fw.op("pool", lambda e, mt=mt: e.tensor_tensor(out=mrg[:], in0=mrg[:], in1=mt[:], op=ALU.add), r=[mrg, mt], w=[mrg])
                            else:
                                fw.op("pool", lambda e, mt=mt, j=j: e.tensor_tensor(out=mT[:, j, :], in0=mrg[:], in1=mt[:], op=ALU.add), r=[mrg, mt], pw=[mT])
                for j in range(8):
                    ps = PS[4 + j % 2]
                    for kc in range(8):
                        fw.op("pe", lambda e, kc=kc, j=j, ps=ps: e.matmul(ps[:], lhsT=wo[:, kc, j * 128:(j + 1) * 128], rhs=mT[:, kc, :], start=(kc == 0), stop=(kc == 7)),
                              r=[wo, mT], pw=[ps])
                    fw.op("act", lambda e, j=j, ps=ps: e.activation(out=ysb[:, j, :], in_=ps[:], func=AF.Copy), r=[ps], pw=[ysb])
                    fw.op("act", lambda e, j=j, ps=ps: e.activation(out=sq[:, j, :], in_=ps[:], func=AF.Square), r=[ps], pw=[sq])
                self.postnorm(xs, ysb, sq, rstd, self.GT1, PS[7], ST)
                fw.dma("pool", self.xT[:, t0:t0 + ST].rearrange("(kc p) t -> p kc t", p=128), xs[:], r=[xs], pw=[self.xT])
                if self.debug:
                    fw.dma("pool", self.s_x1[:, t0:t0 + ST].rearrange("(kc p) t -> p kc t", p=128), xs[:], r=[xs], pw=[self.s_x1])
            fw.barrier()

    def phase_F(self, l):
        nc, fw, sb = self.nc, self.fw, self.sb
        PS = self.PS
        T = self.T
        FT = 256
        NJ = FH // 128
        NT = T // FT
        with ExitStack() as pes:
            w1 = sb(pes, "w1", [128, 8, 2 * FH], BF16)
            w2 = sb(pes, "w2", [128, NJ, D], BF16)
            NG = 2
            HJ = NJ // 2
            g_views = [TT(w1.t, "w1g%d" % i) for i in range(NG)]
            u_views = [TT(w1.t, "w1u%d" % i) for i in range(NG)]
            for gi in range(NG):
                a, b_ = gi * HJ * 128, (gi + 1) * HJ * 128
                for (base, v) in ((0, g_views[gi]), (FH, u_views[gi])):
                    for kc in range(8):
                        fw.dma("pool", w1[:, kc, base + a:base + b_], self.ffn_w1[l, kc * 128:(kc + 1) * 128, base + a:base + b_], pw=[v])
            for jc in range(NJ):
                fw.dma("pool", w2[:, jc, :], self.ffn_w2[l, jc * 128:(jc + 1) * 128, :], pw=[w2])
            xss = [sb(pes, "xsf%d" % i, [128, 8, FT], F32) for i in range(2)]
            hTs = [sb(pes, "hTf%d" % i, [128, 8, FT], BF16) for i in range(2)]
            sqx = sb(pes, "sqx", [128, 8, FT], BF16)
            sqy = sb(pes, "sqy", [128, 8, FT], BF16)
            rstdx = sb(pes, "rstdx", [128, FT], F32)
            rstdy = sb(pes, "rstdy", [128, FT], F32)
            aT = sb(pes, "aT", [128, NJ, FT], BF16)
            ysb = sb(pes, "ysbf", [128, 8, FT], F32)
            sil = [sb(pes, "sil%d" % i, [128, FT], F32) for i in range(2)]
            steps = []
            if l + 1 < self.L:
                wt = [sb(pes, "adawF%d" % i, [128, 8, 128], F32) for i in range(2)]
                steps = self.prep_steps(l + 1, wt, PS[7])
            per_tile = (len(steps) + NT - 1) // NT if steps else 0

            def do_prenorm(ti_):
                xs_ = xss[ti_ % 2]
                fw.dma("sp", xs_[:], self.xT[:, ti_ * FT:(ti_ + 1) * FT].rearrange("(kc p) t -> p kc t", p=128), r=[self.xT], w=[xs_])
                self.prenorm(xs_, hTs[ti_ % 2], sqx, rstdx, self.G2, self.SH2, PS[6], W=FT)

            do_prenorm(0)
            k = 0
            for ti in range(NT):
                t0 = ti * FT
                xs, hT = xss[ti % 2], hTs[ti % 2]
                for _ in range(per_tile):
                    if steps:
                        steps.pop(0)()
                for jc in range(NJ):
                    if jc == 8 and ti + 1 < NT:
                        do_prenorm(ti + 1)
                    psg = PS[(2 * k) % 4]
                    psu = PS[(2 * k + 1) % 4]
                    sl = sil[k % 2]
                    k += 1
                    gv_, uv_ = g_views[jc // HJ], u_views[jc // HJ]
                    for kc in range(8):
                        fw.op("pe", lambda e, kc=kc, jc=jc, psg=psg: e.matmul(psg[:, 0:FT], lhsT=w1[:, kc, jc * 128:(jc + 1) * 128], rhs=hT[:, kc, :], start=(kc == 0), stop=(kc == 7)),
                              r=[gv_, hT], pw=[psg])
                    for kc in range(8):
                        fw.op("pe", lambda e, kc=kc, jc=jc, psu=psu: e.matmul(psu[:, 0:FT], lhsT=w1[:, kc, FH + jc * 128:FH + (jc + 1) * 128], rhs=hT[:, kc, :], start=(kc == 0), stop=(kc == 7)),
                              r=[uv_, hT], pw=[psu])
                    fw.op("act", lambda e, sl=sl, psg=psg: e.activation(out=sl[:], in_=psg[:, 0:FT], func=AF.Silu), r=[psg], w=[sl])
                    fw.op("dve", lambda e, sl=sl, psu=psu, jc=jc: e.tensor_tensor(out=aT[:, jc, :], in0=sl[:], in1=psu[:, 0:FT], op=ALU.mult), r=[sl, psu], pw=[aT])
                for j in range(8):
                    ps = PS[4 + j % 2]
                    for jc in range(NJ):
                        fw.op("pe", lambda e, j=j, jc=jc, ps=ps: e.matmul(ps[:, 0:FT], lhsT=w2[:, jc, j * 128:(j + 1) * 128], rhs=aT[:, jc, :], start=(jc == 0), stop=(jc == NJ - 1)),
                              r=[w2, aT], pw=[ps])
                    fw.op("act", lambda e, j=j, ps=ps: e.activation(out=ysb[:, j, :], in_=ps[:, 0:FT], func=AF.Copy), r=[ps], pw=[ysb])
                    fw.op("act", lambda e, j=j, ps=ps: e.activation(out=sqy[:, j, :], in_=ps[:, 0:FT], func=AF.Square), r=[ps], pw=[sqy])
                self.postnorm(xs, ysb, sqy, rstdy, self.GT2, PS[6], FT)
                fw.dma("pool", self.xT[:, t0:t0 + FT].rearrange("(kc p) t -> p kc t", p=128), xs[:], r=[xs], pw=[self.xT])
            while steps:
                steps.pop(0)()
            fw.barrier()


def _col(v, n):
    return np.ascontiguousarray(v.reshape(n, 128).T)


def prep_inputs(inputs, b, T, L):
    f = lambda a: np.ascontiguousarray(np.asarray(a, dtype=np.float32))
    m = {}
    m["xT"] = f(np.asarray(inputs["x"])[b, :T, :].T)
    m["cT"] = _col(f(inputs["c"])[b], 8)
    m["ada_w"] = f(inputs["ada_w"])[:L]
    m["ada_bT"] = np.stack([_col(f(inputs["ada_b"])[l], 48) for l in range(L)])
    m["vecs"] = np.stack([np.stack([_col(f(inputs[k])[l], 8) for k in ("pre1_w", "post1_w", "pre2_w", "post2_w")], axis=1) for l in range(L)])
    m["w_in"] = f(inputs["w_in"])[:L]
    m["gla_a2"] = f(inputs["gla_a2"])[:L]
    m["gla_ab"] = np.stack([_col(f(inputs["gla_a_b"])[l], 2) for l in range(L)])
    m["gla_nw"] = np.stack([_col(f(inputs["gla_norm_w"])[l], 4) for l in range(L)])
    cw = f(inputs["ml_conv_w"])[:L]
    m["conv_w"] = np.ascontiguousarray(cw.reshape(L, 4, 4, 128).transpose(0, 3, 2, 1))
    m["conv_b"] = np.stack([_col(f(inputs["ml_conv_b"])[l], 4) for l in range(L)])
    m["ml_ifb"] = np.ascontiguousarray(np.stack([f(inputs["ml_i_b"])[:L], f(inputs["ml_f_b"])[:L]], axis=-1))
    dup = lambda a: np.ascontiguousarray(np.concatenate([a, a], axis=1))
    m["s5_lre"] = dup(f(inputs["s5_lam_re"])[:L].transpose(0, 2, 1))
    m["s5_lim"] = dup(f(inputs["s5_lam_im"])[:L].transpose(0, 2, 1))
    m["s5_ls"] = np.ascontiguousarray(np.broadcast_to(f(inputs["s5_log_step"])[:L, None, :], (L, 128, 32)))
    m["s5_bre"] = dup(f(inputs["s5_b_re"])[:L].transpose(0, 2, 1, 3))
    m["s5_bim"] = dup(f(inputs["s5_b_im"])[:L].transpose(0, 2, 1, 3))
    m["s5_cre"] = dup(f(inputs["s5_c_re"])[:L].transpose(0, 3, 1, 2))
    m["s5_cim"] = dup(f(inputs["s5_c_im"])[:L].transpose(0, 3, 1, 2))
    d = f(inputs["s5_d"])[:L].reshape(L, 32, 16)
    m["s5_dblk"] = np.ascontiguousarray(np.broadcast_to(d.transpose(0, 2, 1)[:, None, :, :], (L, 8, 16, 32)).reshape(L, 128, 32))
    m["glu_w"] = f(inputs["s5_glu_w"])[:L]
    m["glu_b"] = np.stack([_col(f(inputs["s5_glu_b"])[l], 4) for l in range(L)])
    m["proj_gla"] = f(inputs["proj_gla"])[:L]
    m["proj_ml"] = f(inputs["proj_ml"])[:L]
    m["proj_s5"] = f(inputs["proj_s5"])[:L]
    m["bgb"] = np.stack([_col(f(inputs["branch_gate_b"])[l], 24) for l in range(L)])
    m["w_out"] = f(inputs["w_out"])[:L]
    m["ffn_w1"] = f(inputs["ffn_w_in"])[:L]
    m["ffn_w2"] = f(inputs["ffn_w_out"])[:L]
    return m


_CACHE = {}


def run(inputs, T=4096, L=2, ncores=8, debug=False):
    key = (T, L, debug)
    if key not in _CACHE:
        _CACHE[key] = Prog(T, L, debug).build()
    nc = _CACHE[key]
    shared = None
    in_maps = []
    for b in range(ncores):
        m = prep_inputs(inputs, b, T, L)
        if shared is None:
            shared = m
        else:
            for k in m:
                if k not in ("xT", "cT"):
                    m[k] = shared[k]
        in_maps.append(m)
    res = run_bass_kernel_spmd(nc, in_maps, core_ids=list(range(ncores)))
    return res.results


def kernel(**inputs):
    results = run(inputs)
    out = np.stack([np.ascontiguousarray(r["outT"].T) for r in results], axis=0)
    return out.astype(np.float32)
```

```python
import math
import numpy as np
from contextlib import ExitStack
import concourse.bass as bass
import concourse.mybir as mybir
from concourse.bass_utils import run_bass_kernel_spmd

F32 = mybir.dt.float32
BF16 = mybir.dt.bfloat16
AF = mybir.ActivationFunctionType
ALU = mybir.AluOpType
AX = mybir.AxisListType

D = 1024
IN_DIM = 6680
FH = 2816
C_GQ, C_GK, C_GV, C_GA, C_GR = 0, 256, 512, 1024, 1040
C_MQ, C_MK, C_MV, C_MI, C_MF, C_MO = 1552, 1808, 2064, 2576, 2580, 2584
C_SU, C_ZG = 3096, 3608
EPS = 1e-6
ST = 512
CH = 128
SL = 64


class Buf:
    __slots__ = ("name", "w", "r")

    def __init__(self, name):
        self.name = name
        self.w = {}
        self.r = {}


class TT:
    def __init__(self, t, name):
        self.t = t
        self.b = Buf(name)

    def __getitem__(self, k):
        return self.t[k]


class FW:
    def __init__(self, nc, es, ndma=12):
        self.nc = nc
        self.eng = {"pe": nc.tensor, "act": nc.scalar, "dve": nc.vector, "pool": nc.gpsimd, "sp": nc.sync}
        self.sem = {}
        self.cnt = {}
        self.seen = {}
        for n in self.eng:
            self.sem[n] = es.enter_context(nc.semaphore("s_" + n))
            self.cnt[n] = 0
            self.seen[n] = {}
        self.dq = {}
        for q in ("sp", "pool"):
            slots = []
            for i in range(ndma):
                key = "d_%s%d" % (q, i)
                self.sem[key] = es.enter_context(nc.semaphore(key))
                slots.append(key)
            self.dq[q] = {"slots": slots, "n": 0, "val": {k: 0 for k in slots}}

    def _wait(self, e, key, val):
        if val <= 0 or self.seen[e].get(key, 0) >= val:
            return
        self.eng[e].wait_ge(self.sem[key], val)
        self.seen[e][key] = val

    def _deps(self, e, reads, writes):
        need = {}
        for t in reads:
            for k, v in t.b.w.items():
                if need.get(k, 0) < v:
                    need[k] = v
        for t in writes:
            for k, v in t.b.w.items():
                if need.get(k, 0) < v:
                    need[k] = v
            for k, v in t.b.r.items():
                if need.get(k, 0) < v:
                    need[k] = v
        for k, v in need.items():
            if e == "pe" and k == "pe":
                continue
            self._wait(e, k, v)

    def op(self, e, fn, r=(), w=(), pw=()):
        self._deps(e, r, tuple(w) + tuple(pw))
        ins = fn(self.eng[e])
        self.cnt[e] += 1
        ins.then_inc(self.sem[e], 1)
        c = self.cnt[e]
        for t in r:
            t.b.r[e] = c
        for t in w:
            t.b.w = {e: c}
            t.b.r = {}
        for t in pw:
            t.b.w[e] = c
        return ins

    def dma(self, q, out, in_, r=(), w=(), pw=(), **kw):
        d = self.dq[q]
        key = d["slots"][d["n"] % len(d["slots"])]
        d["n"] += 1
        self._wait(q, key, d["val"][key])
        self._deps(q, r, tuple(w) + tuple(pw))
        ins = self.eng[q].dma_start(out=out, in_=in_, **kw)
        d["val"][key] += 16
        ins.then_inc(self.sem[key], 16)
        v = d["val"][key]
        for t in r:
            t.b.r[key] = v
        for t in w:
            t.b.w = {key: v}
            t.b.r = {}
        for t in pw:
            t.b.w[key] = v
        return ins

    def barrier(self):
        targets = {n: self.cnt[n] for n in self.eng}
        for q, d in self.dq.items():
            for k, v in d["val"].items():
                targets[k] = v
        for e in self.eng:
            for k, v in targets.items():
                if e == "pe" and k == "pe":
                    continue
                self._wait(e, k, v)


class Prog:
    def __init__(self, T, L, debug=False):
        self.T = T
        self.L = L
        self.debug = debug
        self.NST = T // ST
        self.NCH = T // CH
        self.NTH = T // 8
        self.NSC = T // SL
        self.nc = bass.Bass("TRN2", target_bir_lowering=False)

    def dram_in(self, name, shape, dt=F32):
        return self.nc.dram_tensor(name, list(shape), dt, kind="ExternalInput").ap()

    def dram_scr(self, name, shape, dt):
        kind = "ExternalOutput" if self.debug else "Internal"
        t = TT(self.nc.dram_tensor(name, list(shape), dt, kind=kind).ap(), name)
        return t

    def sb(self, es, name, shape, dt):
        self._uid = getattr(self, "_uid", 0) + 1
        nm = "sb%d_%s" % (self._uid, name)
        return TT(es.enter_context(self.nc.sbuf_tensor(nm, list(shape), dt)), nm)

    def build(self):
        nc = self.nc
        T, L = self.T, self.L
        es = ExitStack()
        with es:
            self.fw = FW(nc, es)
            self.declare_io()
            self.PS = [TT(es.enter_context(nc.psum_tensor("ps%d" % i, [128, 512], F32)), "ps%d" % i) for i in range(8)]
            self.consts(es)
            self.prep_alloc(es)
            import os
            ph = os.environ.get("PHASES", "AS1BF")
            with ExitStack() as pes0:
                wt0 = [self.sb(pes0, "adaw%d" % i, [128, 8, 128], F32) for i in range(3)]
                for stp in self.prep_steps(0, wt0, self.PS[7]):
                    stp()
                self.fw.barrier()
            for l in range(L):
                self.set_layer(l)
                if "A" in ph:
                    self.phase_A(l)
                    self.fw.barrier()
                if "S" in ph:
                    self.phase_S5(l)
                    self.fw.barrier()
                if "1" in ph:
                    self.phase_B1(l)
                    self.fw.barrier()
                if "B" in ph:
                    self.phase_B2(l)
                    self.fw.barrier()
                if "F" in ph:
                    self.phase_F(l)
                    self.fw.barrier()
            self.fw.barrier()
        return nc

    def declare_io(self):
        T, L = self.T, self.L
        di = self.dram_in
        self.xT_in = di("xT", [D, T])
        self.cT = di("cT", [128, 8])
        self.ada_w = di("ada_w", [L, D, 6 * D])
        self.ada_bT = di("ada_bT", [L, 128, 48])
        self.vecs = di("vecs", [L, 128, 4, 8])
        self.w_in = di("w_in", [L, D, IN_DIM])
        self.gla_a2 = di("gla_a2", [L, 16, 256])
        self.gla_ab = di("gla_ab", [L, 128, 2])
        self.gla_nw = di("gla_nw", [L, 128, 4])
        self.conv_w = di("conv_w", [L, 128, 4, 4])
        self.conv_b = di("conv_b", [L, 128, 4])
        self.ml_ifb = di("ml_ifb", [L, 4, 2])
        self.s5_lre = di("s5_lre", [L, 128, 32])
        self.s5_lim = di("s5_lim", [L, 128, 32])
        self.s5_ls = di("s5_ls", [L, 128, 32])
        self.s5_bre = di("s5_bre", [L, 128, 32, 16])
        self.s5_bim = di("s5_bim", [L, 128, 32, 16])
        self.s5_cre = di("s5_cre", [L, 128, 32, 16])
        self.s5_cim = di("s5_cim", [L, 128, 32, 16])
        self.s5_dblk = di("s5_dblk", [L, 128, 32])
        self.glu_w = di("glu_w", [L, 512, 512])
        self.glu_b = di("glu_b", [L, 128, 4])
        self.proj = [di("proj_gla", [L, 512, D]), di("proj_ml", [L, 512, D]), di("proj_s5", [L, 512, D])]
        self.bgb = di("bgb", [L, 128, 24])
        self.w_out = di("w_out", [L, D, D])
        self.ffn_w1 = di("ffn_w1", [L, D, 2 * FH])
        self.ffn_w2 = di("ffn_w2", [L, FH, D])
        self.xT = TT(self.nc.dram_tensor("outT", [D, T], F32, kind="ExternalOutput").ap(), "outT")
        ds = self.dram_scr
        self.s_gq = ds("s_gq", [256, T], BF16)
        self.s_gk = ds("s_gk", [256, T], BF16)
        self.s_g = ds("s_g", [256, T], F32)
        self.s_gr = ds("s_gr", [512, T], BF16)
        self.s_mqk = ds("s_mqk", [512, T], BF16)
        self.s_mi = ds("s_mi", [4, T], F32)
        self.s_mf = ds("s_mf", [4, T], F32)
        self.s_mo = ds("s_mo", [512, T], BF16)
        self.s_gates = ds("s_gates", [3072, T], BF16)
        self.s_gv = ds("s_gv", [T, 512], BF16)
        self.s_mv = ds("s_mv", [T, 512], BF16)
        self.s_ublk = ds("s_ublk", [32, 128, T // 8], BF16)
        self.s_ys5 = ds("s_ys5", [512, T], BF16)
        self.s_kfac = ds("s_kfac", [4, T], F32)
        self.s_floor = ds("s_floor", [4, T], F32)
        if self.debug:
            self.s_ygla = ds("s_ygla", [512, T], BF16)
            self.s_yml = ds("s_yml", [512, T], BF16)
            self.s_x1 = ds("s_x1", [D, T], F32)

    def consts(self, es):
        nc, fw = self.nc, self.fw
        sb = self.sb
        self.identf = sb(es, "identf", [128, 128], F32)
        self.ident = sb(es, "ident", [128, 128], BF16)
        self.ones_bf = sb(es, "ones_bf", [128, 128], BF16)
        self.maskT = sb(es, "maskT", [128, 128], F32)
        self.mask01 = sb(es, "mask01", [128, ST], F32)
        fw.op("pool", lambda e: e.memset(self.identf[:], 0.0), w=[self.identf])
        fw.op("pool", lambda e: e.affine_select(out=self.identf[:], in_=self.identf[:], pattern=[[-1, 128]],
                                                compare_op=ALU.not_equal, fill=1.0, base=0, channel_multiplier=1),
              r=[self.identf], w=[self.identf])
        fw.op("dve", lambda e: e.tensor_copy(out=self.ident[:], in_=self.identf[:]), r=[self.identf], w=[self.ident])
        fw.op("dve", lambda e: e.memset(self.ones_bf[:], 1.0), w=[self.ones_bf])
        fw.op("pool", lambda e: e.memset(self.maskT[:], 1.0), w=[self.maskT])
        fw.op("pool", lambda e: e.affine_select(out=self.maskT[:], in_=self.maskT[:], pattern=[[1, 128]],
                                                compare_op=ALU.is_ge, fill=0.0, base=0, channel_multiplier=-1),
              r=[self.maskT], w=[self.maskT])
        fw.op("dve", lambda e: e.memset(self.mask01[:], 1.0), w=[self.mask01])
        fw.op("dve", lambda e: e.memset(self.mask01[:].rearrange("p (c t) -> p c t", t=CH)[:, :, 0:1], 0.0),
              r=[self.mask01], w=[self.mask01])
        self.selk = []
        self.selv = []
        for hp in range(2):
            t = sb(es, "selk%d" % hp, [4, 128], F32)
            fw.op("pool", lambda e, t=t: e.memset(t[:], 1.0), w=[t])
            fw.op("pool", lambda e, t=t, hp=hp: e.affine_select(out=t[:], in_=t[:], pattern=[[1, 128]], compare_op=ALU.is_ge,
                                                              fill=0.0, base=128 * hp, channel_multiplier=-64), r=[t], w=[t])
            fw.op("pool", lambda e, t=t, hp=hp: e.affine_select(out=t[:], in_=t[:], pattern=[[-1, 128]], compare_op=ALU.is_ge,
                                                              fill=0.0, base=63 - 128 * hp, channel_multiplier=64), r=[t], w=[t])
            self.selk.append(t)
        for h in range(4):
            t = sb(es, "selv%d" % h, [4, 128], F32)
            fw.op("pool", lambda e, t=t: e.memset(t[:], 1.0), w=[t])
            fw.op("pool", lambda e, t=t, h=h: e.affine_select(out=t[:], in_=t[:], pattern=[[0, 128]], compare_op=ALU.is_equal,
                                                            fill=0.0, base=-h, channel_multiplier=1), r=[t], w=[t])
            self.selv.append(t)
        self.eps_t = sb(es, "eps_t", [128, 1], F32)
        fw.op("dve", lambda e: e.memset(self.eps_t[:], EPS), w=[self.eps_t])
        self.nrm_tmp = sb(es, "nrm_tmp", [128, ST], F32)
        self.ln8 = sb(es, "ln8", [128, 1], F32)
        fw.op("dve", lambda e: e.memset(self.ln8[:], math.log(0.125)), w=[self.ln8])
        self.cact = sb(es, "cact", [128, 8], F32)
        fw.dma("sp", self.cact[:], self.cT[:, :], w=[self.cact])
        fw.op("act", lambda e: e.activation(out=self.cact[:], in_=self.cact[:], func=AF.Silu), r=[self.cact], w=[self.cact])
        nchunk = 8
        rows = D // nchunk
        for i in range(nchunk):
            fw.dma("sp", self.xT[i * rows:(i + 1) * rows, :], self.xT_in[i * rows:(i + 1) * rows, :], pw=[self.xT])

    def prep_alloc(self, es):
        sb = self.sb
        self.LP = []
        for i in range(2):
            d = {}
            d["mod"] = sb(es, "mod%d" % i, [128, 48], F32)
            for k in ("G1", "G2", "GT1", "GT2"):
                d[k] = sb(es, "%s_%d" % (k, i), [128, 8], F32)
            d["vec"] = sb(es, "vec%d" % i, [128, 4, 8], F32)
            d["adab"] = sb(es, "adab%d" % i, [128, 48], F32)
            self.LP.append(d)
        self.decay = sb(es, "decay", [4, self.NCH], F32)

    def prep_steps(self, l, wt, ps):
        fw = self.fw
        d = self.LP[l % 2]
        mod, vec, adab = d["mod"], d["vec"], d["adab"]

        def first():
            fw.dma("sp", vec[:], self.vecs[l], w=[vec])
            fw.dma("sp", adab[:], self.ada_bT[l], w=[adab])

        def block(j):
            t = wt[j % len(wt)]
            fw.dma("sp", t[:], self.ada_w[l, :, j * 128:(j + 1) * 128].rearrange("(kc p) m -> p kc m", p=128), w=[t])
            for kc in range(8):
                fw.op("pe", lambda e, kc=kc: e.matmul(ps[:, j:j + 1], lhsT=t[:, kc, :], rhs=self.cact[:, kc:kc + 1], start=(kc == 0), stop=(kc == 7)),
                      r=[t, self.cact], pw=[ps])

        def final():
            fw.op("dve", lambda e: e.tensor_tensor(out=mod[:], in0=ps[:, 0:48], in1=adab[:], op=ALU.add), r=[ps, adab], w=[mod])
            for (k, piece, vi) in (("G1", 1, 0), ("GT1", 2, 1), ("G2", 4, 2), ("GT2", 5, 3)):
                out = d[k]
                fw.op("dve", lambda e, out=out, piece=piece, vi=vi: e.scalar_tensor_tensor(out=out[:], in0=mod[:, piece * 8:(piece + 1) * 8], scalar=1.0, in1=vec[:, vi, :],
                                                                                       op0=ALU.add, op1=ALU.mult), r=[mod, vec], w=[out])
        steps = [first] + [(lambda j=j: block(j)) for j in range(48)] + [final]
        return steps

    def set_layer(self, l):
        d = self.LP[l % 2]
        self.mod = d["mod"]
        self.G1, self.G2, self.GT1, self.GT2 = d["G1"], d["G2"], d["GT1"], d["GT2"]
        mod = self.mod
        self.SH1 = lambda kc: mod[:, 0 + kc:0 + kc + 1]
        self.SH2 = lambda kc: mod[:, 24 + kc:24 + kc + 1]

    def prenorm(self, xs, hT, sq, rstd, G, SH, ps_ss, W=ST):
        fw = self.fw
        fw.op("act", lambda e: e.activation(out=sq[:], in_=xs[:], func=AF.Square), r=[xs], w=[sq])
        for kc in range(8):
            fw.op("pe", lambda e, kc=kc: e.matmul(ps_ss[:, 0:W], lhsT=self.ones_bf[:], rhs=sq[:, kc, :], start=(kc == 0), stop=(kc == 7)),
                  r=[self.ones_bf, sq], pw=[ps_ss])
        fw.op("act", lambda e: e.activation(out=rstd[:], in_=ps_ss[:, 0:W], func=AF.Ln, scale=1.0 / D, bias=self.eps_t[:, 0:1]), r=[ps_ss, self.eps_t], w=[rstd])
        fw.op("act", lambda e: e.activation(out=rstd[:], in_=rstd[:], func=AF.Exp, scale=-0.5), r=[rstd], w=[rstd])
        for kc in range(8):
            fw.op("dve", lambda e, kc=kc: e.scalar_tensor_tensor(out=self.nrm_tmp[:, 0:W], in0=xs[:, kc, :], scalar=G[:, kc:kc + 1], in1=rstd[:],
                                                               op0=ALU.mult, op1=ALU.mult), r=[xs, G, rstd], w=[self.nrm_tmp])
            fw.op("act", lambda e, kc=kc: e.activation(out=hT[:, kc, :], in_=self.nrm_tmp[:, 0:W], func=AF.Identity, bias=SH(kc), scale=1.0),
                  r=[self.nrm_tmp, self.mod], pw=[hT])

    def phase_A(self, l):
        nc, fw, sb = self.nc, self.fw, self.sb
        PS = self.PS
        T = self.T
        with ExitStack() as pes:
            wsb_t = sb(pes, "w_in_sb", [128, 8, IN_DIM], BF16)
            bounds = [0, 2584, 4632, 6680]
            wgroups = [(bounds[i], bounds[i + 1], TT(wsb_t.t, "wg%d" % i)) for i in range(len(bounds) - 1)]

            def wg(c0, M):
                for (a, b_, v) in wgroups:
                    if a <= c0 and c0 + M <= b_:
                        return v
                raise AssertionError("block crosses weight group")

            for gi in range(3):
                a, b_, v = wgroups[gi]
                for kc in range(8):
                    fw.dma("pool", wsb_t[:, kc, a:b_], self.w_in[l, kc * 128:(kc + 1) * 128, a:b_], pw=[v])
            wsb = wsb_t
            a2 = sb(pes, "a2", [16, 256], F32)
            ab = sb(pes, "ab", [128, 2], F32)
            nab = sb(pes, "nab", [128, 2], F32)
            bgb = sb(pes, "bgb", [128, 24], F32)
            fw.dma("sp", a2[:], self.gla_a2[l], w=[a2])
            fw.dma("sp", ab[:], self.gla_ab[l], w=[ab])
            fw.dma("sp", bgb[:], self.bgb[l], w=[bgb])
            fw.op("dve", lambda e: e.tensor_scalar(out=nab[:], in0=ab[:], scalar1=-1.0, scalar2=None, op0=ALU.mult), r=[ab], w=[nab])
            xs = sb(pes, "xs", [128, 8, ST], F32)
            hTs = [sb(pes, "hT%d" % i, [128, 8, ST], BF16) for i in range(2)]
            sq = sb(pes, "sq", [128, 8, ST], BF16)
            rstd = sb(pes, "rstd", [128, ST], F32)
            stg = [sb(pes, "stg%d" % i, [128, ST], BF16) for i in range(6)]
            stgf = [sb(pes, "stgf%d" % i, [128, ST], F32) for i in range(3)]
            z8 = sb(pes, "z8", [64, 32, 128], BF16)
            ubk = sb(pes, "ubk", [128, 32, 64], BF16)
            gaT = sb(pes, "gaT", [16, ST], F32)
            cnt = {"s": 0, "f": 0, "p": 0}

            def nstg():
                cnt["s"] += 1
                return stg[cnt["s"] % 6]

            def nstgf():
                cnt["f"] += 1
                return stgf[cnt["f"] % 3]

            def nps():
                cnt["p"] += 1
                return PS[cnt["p"] % 4]

            cur = {}

            def fm_block(c0, M):
                ps = nps()
                hT = cur["hT"]
                v = wg(c0, M)
                for kc in range(8):
                    fw.op("pe", lambda e, kc=kc: e.matmul(ps[0:M, :], lhsT=wsb[:, kc, c0:c0 + M], rhs=hT[:, kc, :], start=(kc == 0), stop=(kc == 7)),
                          r=[v, hT], pw=[ps])
                return ps

            def do_prenorm(st_):
                fw.dma("sp", xs[:], self.xT[:, st_ * ST:(st_ + 1) * ST].rearrange("(kc p) t -> p kc t", p=128), r=[self.xT], w=[xs])
                self.prenorm(xs, hTs[st_ % 2], sq, rstd, self.G1, self.SH1, PS[4])

            do_prenorm(0)
            for st in range(self.NST):
                t0 = st * ST
                hT = hTs[st % 2]
                cur["hT"] = hT
                for (c0, dst, r0) in [(C_GQ, self.s_gq, 0), (C_GQ + 128, self.s_gq, 128), (C_GK, self.s_gk, 0), (C_GK + 128, self.s_gk, 128),
                                      (C_MQ, self.s_mqk, 0), (C_MQ + 128, self.s_mqk, 128), (C_MK, self.s_mqk, 256), (C_MK + 128, self.s_mqk, 384)]:
                    ps = fm_block(c0, 128)
                    s = nstg()
                    fw.op("dve", lambda e, s=s, ps=ps: e.tensor_copy(out=s[:], in_=ps[:]), r=[ps], w=[s])
                    fw.dma("pool", dst[r0:r0 + 128, t0:t0 + ST], s[:], r=[s], pw=[dst])
                if st + 1 < self.NST:
                    do_prenorm(st + 1)
                for (c0, dst) in [(C_MI, self.s_mi), (C_MF, self.s_mf)]:
                    ps = fm_block(c0, 4)
                    s = nstgf()
                    fw.op("dve", lambda e, s=s, ps=ps: e.tensor_copy(out=s[0:4, :], in_=ps[0:4, :]), r=[ps], w=[s])
                    fw.dma("pool", dst[:, t0:t0 + ST], s[0:4, :], r=[s], pw=[dst])
                ps = fm_block(C_GA, 16)
                fw.op("dve", lambda e, ps=ps: e.tensor_copy(out=gaT[:], in_=ps[0:16, :]), r=[ps], w=[gaT])
                for hp in range(2):
                    ps = nps()
                    fw.op("pe", lambda e, ps=ps, hp=hp: e.matmul(ps[:], lhsT=a2[:, hp * 128:(hp + 1) * 128], rhs=gaT[:], start=True, stop=True),
                          r=[a2, gaT], w=[ps])
                    s = nstgf()
                    fw.op("act", lambda e, ps=ps, s=s, hp=hp: e.activation(out=s[:], in_=ps[:], func=AF.Exp, scale=-1.0, bias=nab[:, hp:hp + 1]),
                          r=[ps, nab], w=[s])
                    fw.op("act", lambda e, s=s: e.activation(out=s[:], in_=s[:], func=AF.Ln, bias=1.0), r=[s], w=[s])
                    fw.op("dve", lambda e, s=s: e.tensor_scalar(out=s[:], in0=s[:], scalar1=-1.0 / 16.0, scalar2=None, op0=ALU.mult), r=[s], w=[s])
                    fw.dma("pool", self.s_g[hp * 128:(hp + 1) * 128, t0:t0 + ST], s[:], r=[s], pw=[self.s_g])
                for j in range(4):
                    ps = fm_block(C_GR + j * 128, 128)
                    s = nstg()
                    fw.op("act", lambda e, s=s, ps=ps: e.activation(out=s[:], in_=ps[:], func=AF.Silu), r=[ps], w=[s])
                    fw.dma("pool", self.s_gr[j * 128:(j + 1) * 128, t0:t0 + ST], s[:], r=[s], pw=[self.s_gr])
                for j in range(4):
                    ps = fm_block(C_MO + j * 128, 128)
                    s = nstg()
                    fw.op("act", lambda e, s=s, ps=ps: e.activation(out=s[:], in_=ps[:], func=AF.Sigmoid), r=[ps], w=[s])
                    fw.dma("pool", self.s_mo[j * 128:(j + 1) * 128, t0:t0 + ST], s[:], r=[s], pw=[self.s_mo])
                for j in range(24):
                    ps = fm_block(C_ZG + j * 128, 128)
                    s = nstg()
                    fw.op("act", lambda e, s=s, ps=ps, j=j: e.activation(out=s[:], in_=ps[:], func=AF.Sigmoid, bias=bgb[:, j:j + 1]), r=[ps, bgb], w=[s])
                    fw.dma("pool", self.s_gates[j * 128:(j + 1) * 128, t0:t0 + ST], s[:], r=[s], pw=[self.s_gates])
                for (c0, dst) in [(C_GV, self.s_gv), (C_MV, self.s_mv)]:
                    for sub in range(4):
                        ps = nps()
                        for kc in range(8):
                            fw.op("pe", lambda e, kc=kc, ps=ps, sub=sub, c0=c0: e.matmul(ps[:], lhsT=hT[:, kc, sub * 128:(sub + 1) * 128], rhs=wsb[:, kc, c0:c0 + 512],
                                                                                 start=(kc == 0), stop=(kc == 7)), r=[wg(c0, 512), hT], pw=[ps])
                        s = nstg()
                        fw.op("dve", lambda e, s=s, ps=ps: e.tensor_copy(out=s[:], in_=ps[:]), r=[ps], w=[s])
                        fw.dma("pool", dst[t0 + sub * 128:t0 + (sub + 1) * 128, :], s[:], r=[s], pw=[dst])
                for jl in range(8):
                    ps = nps()
                    for kc in range(8):
                        fw.op("pe", lambda e, kc=kc, ps=ps, jl=jl: e.matmul(ps[0:64, :], lhsT=hT[:, kc, jl::8], rhs=wsb[:, kc, C_SU:C_SU + 512],
                                                                        start=(kc == 0), stop=(kc == 7)), r=[wg(C_SU, 512), hT], pw=[ps])
                    fw.op("act", lambda e, ps=ps, jl=jl: e.activation(out=z8[:, :, jl * 16:(jl + 1) * 16], in_=ps[0:64, :].rearrange("p (g c) -> p g c", c=16), func=AF.Copy), r=[ps], pw=[z8])
                for gb in range(2):
                    pst = PS[5 + gb]
                    pv = pst[:].bitcast(BF16)
                    for gg in range(16):
                        g = gb * 16 + gg
                        fw.op("pe", lambda e, g=g, gg=gg, pv=pv: e.transpose(pv[:, gg * 64:(gg + 1) * 64], z8[:, g, :], self.ident[0:64, 0:64]),
                              r=[z8, self.ident], pw=[pst])
                    fw.op("dve", lambda e, gb=gb, pv=pv: e.tensor_copy(out=ubk[:, gb * 16:(gb + 1) * 16, :], in_=pv.rearrange("p (g t) -> p g t", t=64)),
                          r=[pst], pw=[ubk])
                th0 = st * (ST // 8)
                fw.dma("pool", self.s_ublk[:, :, th0:th0 + ST // 8].rearrange("g p t -> p g t"), ubk[:], r=[ubk], pw=[self.s_ublk])

    def postnorm(self, xs, ysb, sq, rstd, GT, ps_ss, W):
        fw = self.fw
        for kc in range(8):
            fw.op("pe", lambda e, kc=kc: e.matmul(ps_ss[:, 0:W], lhsT=self.ones_bf[:], rhs=sq[:, kc, :], start=(kc == 0), stop=(kc == 7)),
                  r=[self.ones_bf, sq], pw=[ps_ss])
        fw.op("act", lambda e: e.activation(out=rstd[:], in_=ps_ss[:, 0:W], func=AF.Ln, scale=1.0 / D, bias=self.eps_t[:, 0:1]), r=[ps_ss, self.eps_t], w=[rstd])
        fw.op("act", lambda e: e.activation(out=rstd[:], in_=rstd[:], func=AF.Exp, scale=-0.5), r=[rstd], w=[rstd])
        for kc in range(8):
            fw.op("dve", lambda e, kc=kc: e.tensor_tensor(out=ysb[:, kc, :], in0=ysb[:, kc, :], in1=rstd[:], op=ALU.mult), r=[ysb, rstd], pw=[ysb])
            fw.op("dve", lambda e, kc=kc: e.scalar_tensor_tensor(out=xs[:, kc, :], in0=ysb[:, kc, :], scalar=GT[:, kc:kc + 1], in1=xs[:, kc, :],
                                                               op0=ALU.mult, op1=ALU.add), r=[ysb, GT, xs], pw=[xs])

    def phase_S5(self, l):
        nc, fw, sb = self.nc, self.fw, self.sb
        PS = self.PS
        T, NTH, NSC = self.T, self.NTH, self.NSC
        NT8 = NTH // 128
        MAGIC = 12582912.0
        TWO_PI = 6.28318
        import os
        STOP = int(os.environ.get("S5STOP", "99"))
        with ExitStack() as pes:
            PWre = sb(pes, "PWre", [128, 32, 72], F32)
            PWim = sb(pes, "PWim", [128, 32, 72], F32)
            BA = sb(pes, "BA", [128, 32, 16], F32)
            BB = sb(pes, "BB", [128, 32, 16], F32)
            CA = sb(pes, "CA", [128, 32, 16], F32)
            CB = sb(pes, "CB", [128, 32, 16], F32)
            AR = sb(pes, "AR", [128, 32], F32)
            AIst = sb(pes, "AIst", [128, 32], F32)
            AIsw = sb(pes, "AIsw", [128, 32], F32)
            dblk = sb(pes, "dblk", [128, 32], F32)
            mask8 = sb(pes, "mask8", [128, 128], F32)
            F7 = sb(pes, "F7", [128, 32, 128], BF16)
            Xprev = sb(pes, "Xprev", [128, 32, NSC], BF16)
            fw.dma("sp", dblk[:], self.s5_dblk[l], w=[dblk])
            fw.op("pool", lambda e: e.memset(mask8[:], 1.0), w=[mask8])
            fw.op("pool", lambda e: e.affine_select(out=mask8[:].rearrange("p (i c) -> p i c", c=16), in_=mask8[:].rearrange("p (i c) -> p i c", c=16),
                                                    pattern=[[16, 8], [0, 16]], compare_op=ALU.is_ge, fill=0.0, base=15, channel_multiplier=-1),
                  r=[mask8], w=[mask8])
            with ExitStack() as tes:
                lre = sb(tes, "lre", [128, 32], F32)
                lim = sb(tes, "lim", [128, 32], F32)
                dtt = sb(tes, "dtt", [128, 32], F32)
                rho = sb(tes, "rho", [128, 32], F32)
                th = sb(tes, "th", [128, 32], F32)
                kki = sb(tes, "kki", [128, 72], mybir.dt.int32)
                kk = sb(tes, "kk", [128, 72], F32)
                fw.dma("sp", lre[:], self.s5_lre[l], w=[lre])
                fw.dma("sp", lim[:], self.s5_lim[l], w=[lim])
                fw.dma("sp", dtt[:], self.s5_ls[l], w=[dtt])
                fw.op("act", lambda e: e.activation(out=dtt[:], in_=dtt[:], func=AF.Exp), r=[dtt], w=[dtt])
                fw.op("dve", lambda e: e.tensor_scalar(out=lre[:], in0=lre[:], scalar1=-1e-4, scalar2=None, op0=ALU.min), r=[lre], w=[lre])
                fw.op("dve", lambda e: e.tensor_tensor(out=rho[:], in0=lre[:], in1=dtt[:], op=ALU.mult), r=[lre, dtt], w=[rho])
                fw.op("dve", lambda e: e.tensor_tensor(out=th[:], in0=lim[:], in1=dtt[:], op=ALU.mult), r=[lim, dtt], w=[th])
                fw.op("pool", lambda e: e.iota(kki[:], pattern=[[1, 72]], base=-7, channel_multiplier=0), w=[kki])
                fw.op("dve", lambda e: e.tensor_copy(out=kk[:], in_=kki[:]), r=[kki], w=[kk])
                big = [sb(tes, "big%d" % i, [128, 32, 72], F32) for i in range(4)]
                ph, yy, tt_, ee = big
                B3 = lambda a: a[:, :].unsqueeze(2).to_broadcast([128, 32, 72])
                K3 = kk[:, :].unsqueeze(1).to_broadcast([128, 32, 72])
                fw.op("dve", lambda e: e.tensor_tensor(out=ph[:], in0=B3(th), in1=K3, op=ALU.mult), r=[th, kk], w=[ph])
                fw.op("dve", lambda e: e.tensor_tensor(out=ee[:], in0=B3(rho), in1=K3, op=ALU.mult), r=[rho, kk], w=[ee])
                fw.op("act", lambda e: e.activation(out=ee[:], in_=ee[:], func=AF.Exp), r=[ee], w=[ee])
                for (shift, dst) in [(0.0, PWim), (0.25, PWre)]:
                    fw.op("dve", lambda e, shift=shift: e.tensor_scalar(out=yy[:], in0=ph[:], scalar1=1.0 / (2.0 * math.pi), scalar2=shift, op0=ALU.mult, op1=ALU.add),
                          r=[ph], w=[yy])
                    fw.op("dve", lambda e: e.tensor_scalar(out=tt_[:], in0=yy[:], scalar1=MAGIC, scalar2=None, op0=ALU.add), r=[yy], w=[tt_])
                    fw.op("dve", lambda e: e.tensor_scalar(out=tt_[:], in0=tt_[:], scalar1=-MAGIC, scalar2=None, op0=ALU.add), r=[tt_], w=[tt_])
                    fw.op("dve", lambda e: e.tensor_tensor(out=yy[:], in0=yy[:], in1=tt_[:], op=ALU.subtract), r=[yy, tt_], w=[yy])
                    fw.op("act", lambda e: e.activation(out=yy[:], in_=yy[:], func=AF.Sin, scale=TWO_PI), r=[yy], w=[yy])
                    fw.op("dve", lambda e, dst=dst: e.tensor_tensor(out=dst[:], in0=yy[:], in1=ee[:], op=ALU.mult), r=[yy, ee], w=[dst])
                are = PWre[:, :, 8]
                aim = PWim[:, :, 8]
                am1 = sb(tes, "am1", [128, 32], F32)
                inv = sb(tes, "inv", [128, 32], F32)
                t1 = sb(tes, "t1", [128, 32], F32)
                t2 = sb(tes, "t2", [128, 32], F32)
                zre = sb(tes, "zre", [128, 32], F32)
                zim = sb(tes, "zim", [128, 32], F32)
                V = lambda fn, r, w: fw.op("dve", fn, r=r, w=w)
                V(lambda e: e.tensor_scalar(out=am1[:], in0=are, scalar1=-1.0, scalar2=None, op0=ALU.add), [PWre], [am1])
                V(lambda e: e.tensor_tensor(out=t1[:], in0=lre[:], in1=lre[:], op=ALU.mult), [lre], [t1])
                V(lambda e: e.tensor_tensor(out=t2[:], in0=lim[:], in1=lim[:], op=ALU.mult), [lim], [t2])
                V(lambda e: e.tensor_tensor(out=inv[:], in0=t1[:], in1=t2[:], op=ALU.add), [t1, t2], [inv])
                V(lambda e: e.reciprocal(out=inv[:], in_=inv[:]), [inv], [inv])
                V(lambda e: e.tensor_tensor(out=t1[:], in0=am1[:], in1=lre[:], op=ALU.mult), [am1, lre], [t1])
                V(lambda e: e.tensor_tensor(out=t2[:], in0=aim, in1=lim[:], op=ALU.mult), [PWim, lim], [t2])
                V(lambda e: e.tensor_tensor(out=t1[:], in0=t1[:], in1=t2[:], op=ALU.add), [t1, t2], [t1])
                V(lambda e: e.tensor_tensor(out=zre[:], in0=t1[:], in1=inv[:], op=ALU.mult), [t1, inv], [zre])
                V(lambda e: e.tensor_tensor(out=t1[:], in0=aim, in1=lre[:], op=ALU.mult), [PWim, lre], [t1])
                V(lambda e: e.tensor_tensor(out=t2[:], in0=am1[:], in1=lim[:], op=ALU.mult), [am1, lim], [t2])
                V(lambda e: e.tensor_tensor(out=t1[:], in0=t1[:], in1=t2[:], op=ALU.subtract), [t1, t2], [t1])
                V(lambda e: e.tensor_tensor(out=zim[:], in0=t1[:], in1=inv[:], op=ALU.mult), [t1, inv], [zim])
                bre = sb(tes, "bre", [128, 32, 16], F32)
                bim = sb(tes, "bim", [128, 32, 16], F32)
                cre = sb(tes, "cre", [128, 32, 16], F32)
                cim = sb(tes, "cim", [128, 32, 16], F32)
                u1 = sb(tes, "u1", [128, 32, 16], F32)
                u2 = sb(tes, "u2", [128, 32, 16], F32)
                Bre = sb(tes, "Bre", [128, 32, 16], F32)
                Bim = sb(tes, "Bim", [128, 32, 16], F32)
                fw.dma("sp", bre[:], self.s5_bre[l], w=[bre])
                fw.dma("sp", bim[:], self.s5_bim[l], w=[bim])
                fw.dma("sp", cre[:], self.s5_cre[l], w=[cre])
                fw.dma("sp", cim[:], self.s5_cim[l], w=[cim])
                Z3 = lambda a: a[:, :].unsqueeze(2).to_broadcast([128, 32, 16])
                V(lambda e: e.tensor_tensor(out=u1[:], in0=bre[:], in1=Z3(zre), op=ALU.mult), [bre, zre], [u1])
                V(lambda e: e.tensor_tensor(out=u2[:], in0=bim[:], in1=Z3(zim), op=ALU.mult), [bim, zim], [u2])
                V(lambda e: e.tensor_tensor(out=Bre[:], in0=u1[:], in1=u2[:], op=ALU.subtract), [u1, u2], [Bre])
                V(lambda e: e.tensor_tensor(out=u1[:], in0=bim[:], in1=Z3(zre), op=ALU.mult), [bim, zre], [u1])
                V(lambda e: e.tensor_tensor(out=u2[:], in0=bre[:], in1=Z3(zim), op=ALU.mult), [bre, zim], [u2])
                V(lambda e: e.tensor_tensor(out=Bim[:], in0=u1[:], in1=u2[:], op=ALU.add), [u1, u2], [Bim])
                lo, hi = slice(0, 64), slice(64, 128)
                V(lambda e: e.tensor_copy(out=BA[lo], in_=Bre[lo]), [Bre], [BA])
                fw.op("dve", lambda e: e.tensor_copy(out=BA[hi], in_=Bim[hi]), r=[Bim], pw=[BA])
                V(lambda e: e.tensor_scalar(out=BB[lo], in0=Bim[lo], scalar1=-1.0, scalar2=None, op0=ALU.mult), [Bim], [BB])
                fw.op("dve", lambda e: e.tensor_copy(out=BB[hi], in_=Bre[hi]), r=[Bre], pw=[BB])
                V(lambda e: e.tensor_copy(out=CA[lo], in_=cre[lo]), [cre], [CA])
                fw.op("dve", lambda e: e.tensor_scalar(out=CA[hi], in0=cim[hi], scalar1=-1.0, scalar2=None, op0=ALU.mult), r=[cim], pw=[CA])
                V(lambda e: e.tensor_scalar(out=CB[lo], in0=cim[lo], scalar1=-1.0, scalar2=None, op0=ALU.mult), [cim], [CB])
                fw.op("dve", lambda e: e.tensor_scalar(out=CB[hi], in0=cre[hi], scalar1=-1.0, scalar2=None, op0=ALU.mult), r=[cre], pw=[CB])
                V(lambda e: e.tensor_copy(out=AR[:], in_=PWre[:, :, 71]), [PWre], [AR])
                V(lambda e: e.tensor_scalar(out=AIst[lo], in0=PWim[lo, :, 71], scalar1=-1.0, scalar2=None, op0=ALU.mult), [PWim], [AIst])
                fw.op("dve", lambda e: e.tensor_copy(out=AIst[hi], in_=PWim[hi, :, 71]), r=[PWim], pw=[AIst])
                V(lambda e: e.tensor_scalar(out=AIsw[:], in0=AIst[:], scalar1=-1.0, scalar2=None, op0=ALU.mult), [AIst], [AIsw])
                fw.barrier()
            if STOP <= 1:
                return
            with ExitStack() as p1:
                Sst = sb(p1, "Sst", [128, 32, NSC], F32)
                Ssw = sb(p1, "Ssw", [128, 32, NSC], F32)
                F8 = sb(p1, "F8", [128, 8, 1024], BF16)
                f1 = sb(p1, "f1", [128, 4, 64, 16], F32)
                f2 = sb(p1, "f2", [128, 4, 64, 16], F32)
                Win = sb(p1, "Win", [128, 8, 8, 128], BF16)
                Wsw = sb(p1, "Wsw", [128, 8, 8, 128], BF16)
                U8 = sb(p1, "U8", [128, 8, NTH], BF16)
                for gs in range(4):
                    fw.dma("sp", U8[:], self.s_ublk[gs * 8:(gs + 1) * 8, :, :].rearrange("g p t -> p g t"), r=[self.s_ublk], w=[U8])
                    for q4 in range(2):
                        g0 = gs * 8 + q4 * 4
                        pr = PWre[:, g0:g0 + 4, 70:6:-1].unsqueeze(3).to_broadcast([128, 4, 64, 16])
                        pi = PWim[:, g0:g0 + 4, 70:6:-1].unsqueeze(3).to_broadcast([128, 4, 64, 16])
                        ba = BA[:, g0:g0 + 4, :].unsqueeze(2).to_broadcast([128, 4, 64, 16])
                        bb = BB[:, g0:g0 + 4, :].unsqueeze(2).to_broadcast([128, 4, 64, 16])
                        fw.op("dve", lambda e, pr=pr, ba=ba: e.tensor_tensor(out=f1[:], in0=pr, in1=ba, op=ALU.mult), r=[PWre, BA], w=[f1])
                        fw.op("pool", lambda e, pi=pi, bb=bb: e.tensor_tensor(out=f2[:], in0=pi, in1=bb, op=ALU.mult), r=[PWim, BB], w=[f2])
                        fw.op("dve", lambda e, q4=q4: e.tensor_tensor(out=F8[:, q4 * 4:(q4 + 1) * 4, :].rearrange("p g (j c) -> p g j c", c=16), in0=f1[:], in1=f2[:], op=ALU.add),
                              r=[f1, f2], pw=[F8])
                    fw.op("act", lambda e, gs=gs: e.activation(out=F7[:, gs * 8:(gs + 1) * 8, :], in_=F8[:, :, 56 * 16:64 * 16], func=AF.Copy), r=[F8], pw=[F7])
                    if STOP <= 2:
                        continue
                    for g in range(8):
                        pst = PS[g % 2]
                        pv = pst[:].bitcast(BF16)
                        for jh in range(8):
                            fw.op("pe", lambda e, g=g, jh=jh, pv=pv: e.transpose(pv[:, jh * 128:(jh + 1) * 128], F8[:, g, jh * 128:(jh + 1) * 128], self.ident[:]),
                                  r=[F8, self.ident], pw=[pst])
                        pv3 = pv.rearrange("p (j m) -> p j m", m=128)
                        fw.op("act", lambda e, g=g, pv3=pv3: e.activation(out=Win[:, g, :, :], in_=pv3, func=AF.Copy), r=[pst], pw=[Win])
                        if STOP == 3 and os.environ.get("SUB") == "a":
                            continue
                        fw.op("act", lambda e, g=g, pv3=pv3: e.activation(out=Wsw[:, g, :, 0:64], in_=pv3[:, :, 64:128], func=AF.Copy), r=[pst], pw=[Wsw])
                        fw.op("act", lambda e, g=g, pv3=pv3: e.activation(out=Wsw[:, g, :, 64:128], in_=pv3[:, :, 0:64], func=AF.Copy), r=[pst], pw=[Wsw])
                    if STOP <= 3:
                        continue
                    for (W_, S_, pb) in [(Win, Sst, PS[2]), (Wsw, Ssw, PS[3])]:
                        for g in range(8):
                            for jh in range(8):
                                fw.op("pe", lambda e, g=g, jh=jh, W_=W_, pb=pb: e.matmul(pb[:, g * NSC:(g + 1) * NSC], lhsT=W_[:, g, jh, :], rhs=U8[:, g, jh::8],
                                                                                      start=(jh == 0), stop=(jh == 7)), r=[W_, U8], pw=[pb])
                        fw.op("act", lambda e, S_=S_, pb=pb, gs=gs: e.activation(out=S_[:, gs * 8:(gs + 1) * 8, :], in_=pb[:, 0:8 * NSC].rearrange("p (g n) -> p g n", n=NSC), func=AF.Copy),
                              r=[pb], pw=[S_])
                if STOP <= 4:
                    fw.barrier()
                    return
                xa = [sb(p1, "xa%d" % i, [128, 32], F32) for i in range(2)]
                xb = [sb(p1, "xb%d" % i, [128, 32], F32) for i in range(2)]
                m1 = sb(p1, "m1", [128, 32], F32)
                m2 = sb(p1, "m2", [128, 32], F32)
                m3 = sb(p1, "m3", [128, 32], F32)
                m4 = sb(p1, "m4", [128, 32], F32)
                fw.op("dve", lambda e: e.memset(xa[0][:], 0.0), w=[xa[0]])
                fw.op("dve", lambda e: e.memset(xb[0][:], 0.0), w=[xb[0]])
                for n in range(NSC):
                    ca, cb_ = xa[n % 2], xb[n % 2]
                    na, nb = xa[(n + 1) % 2], xb[(n + 1) % 2]
                    fw.op("act", lambda e, n=n, ca=ca: e.activation(out=Xprev[:, :, n], in_=ca[:], func=AF.Copy), r=[ca], pw=[Xprev])
                    if n == NSC - 1:
                        break
                    fw.op("dve", lambda e, ca=ca: e.tensor_tensor(out=m1[:], in0=ca[:], in1=AR[:], op=ALU.mult), r=[ca, AR], w=[m1])
                    fw.op("dve", lambda e, cb_=cb_: e.tensor_tensor(out=m2[:], in0=cb_[:], in1=AIst[:], op=ALU.mult), r=[cb_, AIst], w=[m2])
                    fw.op("dve", lambda e, cb_=cb_: e.tensor_tensor(out=m3[:], in0=cb_[:], in1=AR[:], op=ALU.mult), r=[cb_, AR], w=[m3])
                    fw.op("dve", lambda e, ca=ca: e.tensor_tensor(out=m4[:], in0=ca[:], in1=AIsw[:], op=ALU.mult), r=[ca, AIsw], w=[m4])
                    fw.op("dve", lambda e: e.tensor_tensor(out=m1[:], in0=m1[:], in1=m2[:], op=ALU.add), r=[m1, m2], w=[m1])
                    fw.op("dve", lambda e: e.tensor_tensor(out=m3[:], in0=m3[:], in1=m4[:], op=ALU.add), r=[m3, m4], w=[m3])
                    fw.op("dve", lambda e, n=n, na=na: e.tensor_tensor(out=na[:], in0=m1[:], in1=Sst[:, :, n], op=ALU.add), r=[m1, Sst], w=[na])
                    fw.op("dve", lambda e, n=n, nb=nb: e.tensor_tensor(out=nb[:], in0=m3[:], in1=Ssw[:, :, n], op=ALU.add), r=[m3, Ssw], w=[nb])
                fw.barrier()
            if STOP <= 5:
                return
            with ExitStack() as p2:
                G8 = sb(p2, "G8", [128, 8, 72 * 16], BF16)
                g1 = sb(p2, "g1", [128, 2, 72, 16], F32)
                g2 = sb(p2, "g2", [128, 2, 72, 16], F32)
                Wi = sb(p2, "Wi", [128, 8, 1024], BF16)
                U8 = sb(p2, "U8b", [128, 8, NTH], BF16)
                Yact = sb(p2, "Yact", [128, 8, NTH], BF16)
                Y8 = sb(p2, "Y8", [128, NT8, 8, 512], BF16)
                ysT = sb(p2, "ysT", [128, 4, T], BF16)
                ylin = [sb(p2, "ylin%d" % i, [128, NTH], F32) for i in range(2)]
                gw = sb(p2, "gw", [128, 4, 512], BF16)
                gb = sb(p2, "gb", [128, 4], F32)
                fw.dma("pool", gw[:], self.glu_w[l].rearrange("(kc p) m -> p kc m", p=128), w=[gw])
                fw.dma("sp", gb[:], self.glu_b[l], w=[gb])
                for gs in range(4):
                    fw.dma("sp", U8[:], self.s_ublk[gs * 8:(gs + 1) * 8, :, :].rearrange("g p t -> p g t"), r=[self.s_ublk], w=[U8])
                    for q2 in range(4):
                        g0 = gs * 8 + q2 * 2
                        pr = PWre[:, g0:g0 + 2, :].unsqueeze(3).to_broadcast([128, 2, 72, 16])
                        pi = PWim[:, g0:g0 + 2, :].unsqueeze(3).to_broadcast([128, 2, 72, 16])
                        ca = CA[:, g0:g0 + 2, :].unsqueeze(2).to_broadcast([128, 2, 72, 16])
                        cb2 = CB[:, g0:g0 + 2, :].unsqueeze(2).to_broadcast([128, 2, 72, 16])
                        fw.op("dve", lambda e, pr=pr, ca=ca: e.tensor_tensor(out=g1[:], in0=pr, in1=ca, op=ALU.mult), r=[PWre, CA], w=[g1])
                        fw.op("pool", lambda e, pi=pi, cb2=cb2: e.tensor_tensor(out=g2[:], in0=pi, in1=cb2, op=ALU.mult), r=[PWim, CB], w=[g2])
                        fw.op("dve", lambda e, q2=q2: e.tensor_tensor(out=G8[:, q2 * 2:(q2 + 1) * 2, :].rearrange("p g (j c) -> p g j c", c=16), in0=g1[:], in1=g2[:], op=ALU.add),
                              r=[g1, g2], pw=[G8])
                    if STOP <= 6:
                        continue
                    for g in range(8):
                        gi = gs * 8 + g
                        pa, pb = PS[0 + 2 * (g % 2)], PS[1 + 2 * (g % 2)]
                        fw.op("pe", lambda e, g=g, gi=gi, pa=pa: e.matmul(pa[:], lhsT=F7[:, gi, :], rhs=G8[:, g, 0:512], start=True, stop=True), r=[F7, G8], w=[pa])
                        fw.op("pe", lambda e, g=g, gi=gi, pb=pb: e.matmul(pb[:], lhsT=F7[:, gi, :], rhs=G8[:, g, 512:1024], start=True, stop=True), r=[F7, G8], w=[pb])
                        fw.op("dve", lambda e, g=g, pa=pa: e.tensor_tensor(out=Wi[:, g, 0:128], in0=pa[:, 0:128], in1=mask8[:], op=ALU.mult), r=[pa, mask8], pw=[Wi])
                        fw.op("act", lambda e, g=g, pa=pa: e.activation(out=Wi[:, g, 128:512], in_=pa[:, 128:512], func=AF.Copy), r=[pa], pw=[Wi])
                        fw.op("act", lambda e, g=g, pb=pb: e.activation(out=Wi[:, g, 512:1024], in_=pb[:], func=AF.Copy), r=[pb], pw=[Wi])
                    if STOP <= 7:
                        continue
                    for g in range(8):
                        gi = gs * 8 + g
                        yps = PS[4 + (g % 2)]
                        y3 = yps[:, 0:NTH].rearrange("p (n i) -> p n i", i=8)
                        u3 = U8[:, g, :].rearrange("p (n i) -> p n i", i=8)
                        for d in range(8):
                            fw.op("pe", lambda e, g=g, d=d, y3=y3, u3=u3: e.matmul(y3[:, :, d:8], lhsT=Wi[:, g, d * 128:(d + 1) * 128], rhs=u3[:, :, 0:8 - d],
                                                                                 start=(d == 0), stop=False), r=[Wi, U8], pw=[yps])
                        for ih in range(8):
                            fw.op("pe", lambda e, g=g, gi=gi, ih=ih, y3=y3: e.matmul(y3[:, :, ih], lhsT=G8[:, g, (ih + 1) * 128:(ih + 2) * 128], rhs=Xprev[:, gi, :],
                                                                                   start=False, stop=(ih == 7)), r=[G8, Xprev], pw=[yps])
                        yl = ylin[g % 2]
                        fw.op("dve", lambda e, g=g, gi=gi, yl=yl, yps=yps: e.scalar_tensor_tensor(out=yl[:], in0=U8[:, g, :], scalar=dblk[:, gi:gi + 1], in1=yps[:, 0:NTH],
                                                                                               op0=ALU.mult, op1=ALU.add), r=[U8, dblk, yps], w=[yl])
                        fw.op("act", lambda e, g=g, yl=yl: e.activation(out=Yact[:, g, :], in_=yl[:], func=AF.Gelu), r=[yl], pw=[Yact])
                    if STOP <= 8:
                        continue
                    for thi in range(NT8):
                        pst = PS[6 + (thi % 2)]
                        pv = pst[:].bitcast(BF16)
                        for g in range(8):
                            fw.op("pe", lambda e, g=g, thi=thi, pv=pv: e.transpose(pv[:, g * 128:(g + 1) * 128], Yact[:, g, thi * 128:(thi + 1) * 128], self.ident[:]),
                                  r=[Yact, self.ident], pw=[pst])
                        fw.op("act", lambda e, thi=thi, pv=pv, gs=gs: e.activation(out=Y8[:, thi, :, gs * 128:(gs + 1) * 128].rearrange("p i (g c) -> p i g c", c=16),
                                                                             in_=pv.rearrange("p (g i c) -> p i g c", i=8, c=16), func=AF.Copy), r=[pst], pw=[Y8])
                if STOP <= 9:
                    fw.barrier()
                    return
                k = 0
                for thi in range(NT8):
                    for kc in range(4):
                        pst = PS[6 + (k % 2)]
                        k += 1
                        pv = pst[:].bitcast(BF16)
                        for il in range(8):
                            fw.op("pe", lambda e, il=il, thi=thi, kc=kc, pv=pv: e.transpose(pv[:, il * 128:(il + 1) * 128], Y8[:, thi, il, kc * 128:(kc + 1) * 128], self.ident[:]),
                                  r=[Y8, self.ident], pw=[pst])
                        eng = "act"
                        if eng == "dve":
                            fw.op("dve", lambda e, thi=thi, kc=kc, pv=pv: e.tensor_copy(out=ysT[:, kc, thi * 1024:(thi + 1) * 1024].rearrange("p (t i) -> p t i", i=8),
                                                                                 in_=pv.rearrange("p (i t) -> p t i", t=128)), r=[pst], pw=[ysT])
                        else:
                            fw.op("act", lambda e, thi=thi, kc=kc, pv=pv: e.activation(out=ysT[:, kc, thi * 1024:(thi + 1) * 1024].rearrange("p (t i) -> p t i", i=8),
                                                                                in_=pv.rearrange("p (i t) -> p t i", t=128), func=AF.Copy), r=[pst], pw=[ysT])
                stg = [sb(p2, "gstg%d" % i, [128, ST], BF16) for i in range(3)]
                gsg = [sb(p2, "gsg%d" % i, [128, ST], BF16) for i in range(2)]
                k = 0
                for st in range(self.NST):
                    t0 = st * ST
                    for jc in range(4):
                        ps = PS[k % 4]
                        for kc in range(4):
                            fw.op("pe", lambda e, kc=kc, jc=jc, ps=ps, t0=t0: e.matmul(ps[:], lhsT=gw[:, kc, jc * 128:(jc + 1) * 128], rhs=ysT[:, kc, t0:t0 + ST],
                                                                                   start=(kc == 0), stop=(kc == 3)), r=[gw, ysT], pw=[ps])
                        sg = gsg[k % 2]
                        so = stg[k % 3]
                        k += 1
                        fw.op("act", lambda e, sg=sg, ps=ps, jc=jc: e.activation(out=sg[:], in_=ps[:], func=AF.Sigmoid, bias=gb[:, jc:jc + 1]), r=[ps, gb], w=[sg])
                        fw.op("dve", lambda e, sg=sg, so=so, jc=jc, t0=t0: e.tensor_tensor(out=so[:], in0=sg[:], in1=ysT[:, jc, t0:t0 + ST], op=ALU.mult), r=[sg, ysT], w=[so])
                        fw.dma("pool", self.s_ys5[jc * 128:(jc + 1) * 128, t0:t0 + ST], so[:], r=[so], pw=[self.s_ys5])
                fw.barrier()

    def phase_B1(self, l):
        nc, fw, sb = self.nc, self.fw, self.sb
        T, NCH = self.T, self.NCH
        with ExitStack() as pes:
            A = sb(pes, "b1A", [4, T], F32)
            B = sb(pes, "b1B", [4, T], F32)
            C = sb(pes, "b1C", [4, T], F32)
            M = sb(pes, "b1M", [4, T], F32)
            ifb = sb(pes, "ifb", [4, 2], F32)
            nfb = sb(pes, "nfb", [4, 1], F32)
            umax = sb(pes, "umax", [4, NCH], F32)
            blast = sb(pes, "blast", [4, NCH], F32)
            mnext = sb(pes, "mnext", [4, NCH], F32)
            mprev = sb(pes, "mprev", [4, NCH], F32)
            Rr = sb(pes, "Rr", [4, NCH], F32)
            fw.dma("sp", A[:], self.s_mi[:, :], r=[self.s_mi], w=[A])
            fw.dma("sp", B[:], self.s_mf[:, :], r=[self.s_mf], w=[B])
            fw.dma("sp", ifb[:], self.ml_ifb[l], w=[ifb])
            V = lambda fn, r, w: fw.op("dve", fn, r=r, w=w)
            V(lambda e: e.tensor_scalar(out=nfb[:], in0=ifb[:, 1:2], scalar1=-1.0, scalar2=None, op0=ALU.mult), [ifb], [nfb])
            V(lambda e: e.memset(M[:], 1.0), [], [M])
            V(lambda e: e.memset(M[:].rearrange("p (c t) -> p c t", t=CH)[:, :, 0:1], 0.0), [M], [M])
            V(lambda e: e.tensor_scalar(out=A[:], in0=A[:], scalar1=ifb[:, 0:1], scalar2=None, op0=ALU.add), [A, ifb], [A])
            fw.op("act", lambda e: e.activation(out=B[:], in_=B[:], func=AF.Exp, scale=-1.0, bias=nfb[:, 0:1]), r=[B, nfb], w=[B])
            fw.op("act", lambda e: e.activation(out=B[:], in_=B[:], func=AF.Ln, bias=1.0), r=[B], w=[B])
            V(lambda e: e.tensor_tensor_scan(out=C[:], data0=M[:], data1=B[:], initial=0.0, op0=ALU.mult, op1=ALU.add), [M, B], [C])
            V(lambda e: e.tensor_tensor(out=A[:], in0=A[:], in1=C[:], op=ALU.add), [A, C], [A])
            V(lambda e: e.tensor_reduce(out=umax[:], in_=A[:].rearrange("p (c t) -> p c t", t=CH), axis=AX.X, op=ALU.max), [A], [umax])
            V(lambda e: e.tensor_scalar(out=blast[:], in0=C[:, CH - 1::CH], scalar1=-1.0, scalar2=None, op0=ALU.mult), [C], [blast])
            V(lambda e: e.tensor_tensor_scan(out=mnext[:], data0=umax[:], data1=blast[:], initial=0.0, op0=ALU.max, op1=ALU.add), [umax, blast], [mnext])
            V(lambda e: e.tensor_tensor(out=Rr[:], in0=mnext[:], in1=blast[:], op=ALU.subtract), [mnext, blast], [Rr])
            V(lambda e: e.memset(mprev[:], 0.0), [], [mprev])
            if NCH > 1:
                V(lambda e: e.tensor_copy(out=mprev[:, 1:NCH], in_=mnext[:, 0:NCH - 1]), [mnext, mprev], [mprev])
            V(lambda e: e.tensor_tensor(out=mprev[:], in0=mprev[:], in1=Rr[:], op=ALU.subtract), [mprev, Rr], [mprev])
            fw.op("act", lambda e: e.activation(out=self.decay[:], in_=mprev[:], func=AF.Exp), r=[mprev], w=[self.decay])
            R3 = Rr[:, :].unsqueeze(2).to_broadcast([4, NCH, CH])
            V(lambda e: e.tensor_tensor(out=A[:].rearrange("p (c t) -> p c t", t=CH), in0=A[:].rearrange("p (c t) -> p c t", t=CH), in1=R3, op=ALU.subtract), [A, Rr], [A])
            fw.op("act", lambda e: e.activation(out=A[:], in_=A[:], func=AF.Exp, bias=self.ln8[0:4, 0:1]), r=[A, self.ln8], w=[A])
            V(lambda e: e.tensor_tensor(out=C[:].rearrange("p (c t) -> p c t", t=CH), in0=C[:].rearrange("p (c t) -> p c t", t=CH), in1=R3, op=ALU.subtract), [C, Rr], [C])
            fw.op("act", lambda e: e.activation(out=C[:], in_=C[:], func=AF.Exp), r=[C], w=[C])
            fw.dma("sp", self.s_kfac[:, :], A[:], r=[A], w=[self.s_kfac])
            fw.dma("sp", self.s_floor[:, :], C[:], r=[C], w=[self.s_floor])
            fw.barrier()

    def phase_B2(self, l):
        nc, fw, sb = self.nc, self.fw, self.sb
        PS = self.PS
        T, NCH = self.T, self.NCH
        with ExitStack() as pes:
            pj = [sb(pes, "pj%d" % i, [128, 4, D], BF16) for i in range(3)]
            wo = sb(pes, "wo", [128, 8, D], BF16)
            for i in range(3):
                fw.dma("pool", pj[i][:], self.proj[i][l].rearrange("(kc p) m -> p kc m", p=128), w=[pj[i]])
            for kc in range(8):
                fw.dma("pool", wo[:, kc, :], self.w_out[l, kc * 128:(kc + 1) * 128, :], pw=[wo])
            gnw = sb(pes, "gnw", [128, 4], F32)
            cw = sb(pes, "cw", [128, 4, 4], F32)
            cb = sb(pes, "cb", [128, 4], F32)
            fw.dma("sp", gnw[:], self.gla_nw[l], w=[gnw])
            fw.dma("sp", cw[:], self.conv_w[l], w=[cw])
            fw.dma("sp", cb[:], self.conv_b[l], w=[cb])
            decb = sb(pes, "decb", [128, 2, NCH], F32)
            for hp in range(2):
                fw.op("pe", lambda e, hp=hp: e.matmul(PS[7][:, 0:NCH], lhsT=self.selk[hp][:], rhs=self.decay[:], start=True, stop=True), r=[self.selk[hp], self.decay], w=[PS[7]])
                fw.op("dve", lambda e, hp=hp: e.tensor_copy(out=decb[:, hp, :], in_=PS[7][:, 0:NCH]), r=[PS[7]], pw=[decb])
            gq = sb(pes, "gq", [128, 2, ST], BF16)
            gk = sb(pes, "gk", [128, 2, ST], BF16)
            gg = sb(pes, "gg", [128, 2, ST], F32)
            gr = sb(pes, "gr", [128, 4, ST], BF16)
            gv = sb(pes, "gv", [128, 4, 512], BF16)
            mqk = sb(pes, "mqk", [128, 4, ST + 3], BF16)
            mo = sb(pes, "mo", [128, 4, ST], BF16)
            mv = sb(pes, "mv", [128, 4, 512], BF16)
            kfac = sb(pes, "kfac", [4, ST], F32)
            flo = sb(pes, "flo", [4, ST], F32)
            gts = [sb(pes, "gates%d" % i, [128, 3, ST], BF16) for i in range(2)]
            ys5 = sb(pes, "ys5", [128, 4, ST], BF16)
            xs = sb(pes, "xsb", [128, 8, ST], F32)
            cum = sb(pes, "cum", [128, ST], F32)
            eq = sb(pes, "eq", [128, ST], F32)
            ek = sb(pes, "ek", [128, ST], F32)
            ekh = sb(pes, "ekh", [128, ST], F32)
            ecl = sb(pes, "ecl", [128, 2, 4], F32)
            qt = sb(pes, "qt", [128, 2, ST], BF16)
            kt = sb(pes, "kt", [128, 2, ST], BF16)
            kh = sb(pes, "kh", [128, 2, ST], BF16)
            khTM = sb(pes, "khTM", [128, 4, 256], BF16)
            ATs = [sb(pes, "AT%d" % i, [128, 128], BF16) for i in range(3)]
            S = sb(pes, "S", [128, 2, 128], F32)
            Sbf = sb(pes, "Sbf", [128, 2, 128], BF16)
            osqs = [sb(pes, "osq%d" % i, [128, ST], BF16) for i in range(2)]
            rss = [sb(pes, "rs%d" % i, [128, ST], F32) for i in range(2)]
            tmps = [sb(pes, "tmpb%d" % i, [128, ST], F32) for i in range(2)]
            ygla = sb(pes, "ygla", [128, 4, ST], BF16)
            yml = sb(pes, "yml", [128, 4, ST], BF16)
            acc = sb(pes, "acc", [128, ST], F32)
            mqs = sb(pes, "mqs", [128, 2, ST], BF16)
            mks = sb(pes, "mks", [128, ST], F32)
            ktl = sb(pes, "ktl", [128, 2, ST], BF16)
            ktlTM = sb(pes, "ktlTM", [128, 4, 256], BF16)
            ksum = sb(pes, "ksum", [128, 2, 4], F32)
            mem = sb(pes, "mem", [128, 2, 128], F32)
            memS = sb(pes, "memS", [128, 2, 128], F32)
            memSbf = sb(pes, "memSbf", [128, 2, 128], BF16)
            nrm = sb(pes, "nrm", [128, 2], F32)
            nrmS = sb(pes, "nrmS", [128, 2], F32)
            nbc = sb(pes, "nbc", [128, 2, 128], BF16)
            fls = sb(pes, "fls", [128, ST], F32)
            mrgs = [sb(pes, "mrg%d" % i, [128, ST], F32) for i in range(2)]
            mtmps = [[sb(pes, "mtmp%d_%d" % (q, i), [128, ST], F32) for i in range(2)] for q in range(2)]
            mT = sb(pes, "mT", [128, 8, ST], BF16)
            ysb = sb(pes, "ysb", [128, 8, ST], F32)
            sq = sb(pes, "sqb", [128, 8, ST], BF16)
            rstd = sb(pes, "rstdb", [128, ST], F32)
            for t_ in (S, Sbf, mem, nrm):
                fw.op("dve", lambda e, t_=t_: e.memset(t_[:], 0.0), w=[t_])
            cntr = {"at": 0, "ps": 0}
            ATV = [TT(PS[6].t, "atv0"), TT(PS[6].t, "atv1")]
            STV = TT(PS[6].t, "stv")
            tmp2 = sb(pes, "tmp2b", [128, ST], F32)

            def chunk_attention(c, h, kT_t, qT_t, v_tm, st_bf, o_ps, den=None):
                hp, off = h // 2, 64 * (h % 2)
                cs = slice(c * CH, (c + 1) * CH)
                cntr["at"] += 1
                aps = PS[6]
                AT = ATs[cntr["at"] % 3]
                fw.op("pe", lambda e: e.matmul(aps[:, 0:CH], lhsT=kT_t[off:off + 64, hp, cs], rhs=qT_t[off:off + 64, hp, cs], start=True, stop=True),
                      r=[kT_t, qT_t], w=[aps])
                fw.op("dve", lambda e: e.tensor_tensor(out=AT[:], in0=aps[:, 0:CH], in1=self.maskT[:], op=ALU.mult), r=[aps, self.maskT], w=[AT])
                fw.op("pe", lambda e: e.matmul(o_ps[:, cs], lhsT=v_tm[:, c, h * 128:(h + 1) * 128], rhs=AT[:], start=True, stop=False), r=[v_tm, AT], pw=[o_ps])
                fw.op("pe", lambda e: e.matmul(o_ps[:, cs], lhsT=st_bf[off:off + 64, hp, :], rhs=qT_t[off:off + 64, hp, cs], start=False, stop=True),
                      r=[st_bf, qT_t], pw=[o_ps])
                if den is not None:
                    d_ps, nb_ = den
                    fw.op("pe", lambda e: e.matmul(d_ps[:, cs], lhsT=self.ones_bf[:], rhs=AT[:], start=True, stop=False), r=[self.ones_bf, AT], pw=[d_ps])
                    fw.op("pe", lambda e: e.matmul(d_ps[:, cs], lhsT=nb_[off:off + 64, hp, :], rhs=qT_t[off:off + 64, hp, cs], start=False, stop=True),
                          r=[nb_, qT_t], pw=[d_ps])

            def issue_chain_loads(st):
                t0 = st * ST
                if True:
                    fw.dma("sp", gq[:], self.s_gq[:, t0:t0 + ST].rearrange("(a p) t -> p a t", p=128), r=[self.s_gq], w=[gq])
                    fw.dma("sp", gk[:], self.s_gk[:, t0:t0 + ST].rearrange("(a p) t -> p a t", p=128), r=[self.s_gk], w=[gk])
                    fw.dma("sp", gg[:], self.s_g[:, t0:t0 + ST].rearrange("(a p) t -> p a t", p=128), r=[self.s_g], w=[gg])
                    fw.dma("sp", gr[:], self.s_gr[:, t0:t0 + ST].rearrange("(a p) t -> p a t", p=128), r=[self.s_gr], w=[gr])
                    fw.dma("sp", gv[:], self.s_gv[t0:t0 + ST, :].rearrange("(a p) m -> p a m", p=128), r=[self.s_gv], w=[gv])
                    if st == 0:
                        fw.op("dve", lambda e: e.memset(mqk[:, :, 0:3], 0.0), w=[mqk])
                        fw.dma("sp", mqk[:, :, 3:ST + 3], self.s_mqk[:, 0:ST].rearrange("(a p) t -> p a t", p=128), r=[self.s_mqk], pw=[mqk])
                    else:
                        fw.dma("sp", mqk[:], self.s_mqk[:, t0 - 3:t0 + ST].rearrange("(a p) t -> p a t", p=128), r=[self.s_mqk], w=[mqk])
                    fw.dma("sp", mo[:], self.s_mo[:, t0:t0 + ST].rearrange("(a p) t -> p a t", p=128), r=[self.s_mo], w=[mo])
                    fw.dma("sp", mv[:], self.s_mv[t0:t0 + ST, :].rearrange("(a p) m -> p a m", p=128), r=[self.s_mv], w=[mv])
                    fw.dma("sp", kfac[:], self.s_kfac[:, t0:t0 + ST], r=[self.s_kfac], w=[kfac])
                    fw.dma("sp", flo[:], self.s_floor[:, t0:t0 + ST], r=[self.s_floor], w=[flo])

            lo, hi = slice(0, 64), slice(64, 128)
            import os
            BSTOP = float(os.environ.get("B2STOP", "99"))
            for st in range(self.NST):
                t0 = st * ST
                L_ = lambda dst, src, rr: fw.dma("sp", dst, src, r=[rr], w=[])
                if st == 0:
                    issue_chain_loads(0)
                fw.dma("sp", ys5[:], self.s_ys5[:, t0:t0 + ST].rearrange("(a p) t -> p a t", p=128), r=[self.s_ys5], w=[ys5])
                fw.dma("sp", xs[:], self.xT[:, t0:t0 + ST].rearrange("(kc p) t -> p kc t", p=128), r=[self.xT], w=[xs])
                for hp in range(2):
                    fw.op("dve", lambda e, hp=hp: e.tensor_tensor_scan(out=cum[:], data0=self.mask01[:], data1=gg[:, hp, :], initial=0.0, op0=ALU.mult, op1=ALU.add),
                          r=[self.mask01, gg], w=[cum])
                    fw.op("act", lambda e: e.activation(out=eq[:], in_=cum[:], func=AF.Exp), r=[cum], w=[eq])
                    fw.op("dve", lambda e, hp=hp: e.scalar_tensor_tensor(out=qt[:, hp, :], in0=gq[:, hp, :], scalar=0.125, in1=eq[:], op0=ALU.mult, op1=ALU.mult),
                          r=[gq, eq], pw=[qt])
                    fw.op("act", lambda e: e.activation(out=ek[:], in_=cum[:], func=AF.Exp, scale=-1.0), r=[cum], w=[ek])
                    fw.op("dve", lambda e, hp=hp: e.tensor_tensor(out=kt[:, hp, :], in0=gk[:, hp, :], in1=ek[:], op=ALU.mult), r=[gk, ek], pw=[kt])
                    for c in range(4):
                        fw.op("act", lambda e, c=c: e.activation(out=ekh[:, c * CH:(c + 1) * CH], in_=cum[:, c * CH:(c + 1) * CH], func=AF.Exp, scale=-1.0,
                                                                 bias=cum[:, c * CH + CH - 1:c * CH + CH]), r=[cum], pw=[ekh])
                    fw.op("dve", lambda e, hp=hp: e.tensor_tensor(out=kh[:, hp, :], in0=gk[:, hp, :], in1=ekh[:], op=ALU.mult), r=[gk, ekh], pw=[kh])
                    fw.op("act", lambda e, hp=hp: e.activation(out=ecl[:, hp, :], in_=cum[:, CH - 1::CH], func=AF.Exp), r=[cum], pw=[ecl])
                    pv = PS[7][:].bitcast(BF16)
                    for c in range(4):
                        fw.op("pe", lambda e, c=c, hp=hp, pv=pv: e.transpose(pv[:, c * 128:(c + 1) * 128], kh[:, hp, c * CH:(c + 1) * CH], self.ident[:]),
                              r=[kh, self.ident], pw=[PS[7]])
                    fw.op("act", lambda e, hp=hp, pv=pv: e.activation(out=khTM[:, :, hp * 128:(hp + 1) * 128], in_=pv[:, 0:512].rearrange("p (c m) -> p c m", m=128), func=AF.Copy),
                          r=[PS[7]], pw=[khTM])
                for ti in range(4):
                    fw.op("dve", lambda e, ti=ti: e.tensor_scalar(out=acc[:], in0=mqk[:, ti, 3:ST + 3], scalar1=cw[:, ti, 3:4], scalar2=cb[:, ti:ti + 1], op0=ALU.mult, op1=ALU.add),
                          r=[mqk, cw, cb], w=[acc])
                    for k in (2, 1, 0):
                        fw.op("dve", lambda e, ti=ti, k=k: e.scalar_tensor_tensor(out=acc[:], in0=mqk[:, ti, k:ST + k], scalar=cw[:, ti, k:k + 1], in1=acc[:], op0=ALU.mult, op1=ALU.add),
                              r=[mqk, cw, acc], w=[acc])
                    if ti < 2:
                        fw.op("act", lambda e, ti=ti: e.activation(out=mqs[:, ti, :], in_=acc[:], func=AF.Silu), r=[acc], pw=[mqs])
                    else:
                        hp = ti - 2
                        fw.op("act", lambda e: e.activation(out=mks[:], in_=acc[:], func=AF.Silu), r=[acc], w=[mks])
                        fw.op("pe", lambda e, hp=hp: e.matmul(PS[7][:], lhsT=self.selk[hp][:], rhs=kfac[:], start=True, stop=True), r=[self.selk[hp], kfac], w=[PS[7]])
                        fw.op("dve", lambda e, hp=hp: e.tensor_tensor(out=ktl[:, hp, :], in0=mks[:], in1=PS[7][:], op=ALU.mult), r=[mks, PS[7]], pw=[ktl])
                        fw.op("dve", lambda e, hp=hp: e.tensor_reduce(out=ksum[:, hp, :], in_=ktl[:, hp, :].rearrange("p (c t) -> p c t", t=CH), axis=AX.X, op=ALU.add),
                              r=[ktl], pw=[ksum])
                        pv = PS[7][:].bitcast(BF16)
                        for c in range(4):
                            fw.op("pe", lambda e, c=c, hp=hp, pv=pv: e.transpose(pv[:, c * 128:(c + 1) * 128], ktl[:, hp, c * CH:(c + 1) * CH], self.ident[:]),
                                  r=[ktl, self.ident], pw=[PS[7]])
                        fw.op("act", lambda e, hp=hp, pv=pv: e.activation(out=ktlTM[:, :, hp * 128:(hp + 1) * 128], in_=pv[:, 0:512].rearrange("p (c m) -> p c m", m=128), func=AF.Copy),
                              r=[PS[7]], pw=[ktlTM])
                for hp in range(2):
                    gla_o = [PS[0], PS[1]]
                    ml_o = [PS[2], PS[3]]
                    ml_d = [PS[4], PS[5]]
                    for c in range(4):
                        cg = st * 4 + c
                        for hh in range(2):
                            h = hp * 2 + hh
                            chunk_attention(c, h, kt, qt, gv, Sbf, gla_o[hh])
                            fw.op("pe", lambda e, c=c, h=h, hh=hh, hp=hp: e.matmul(PS[7][:, hh * 128:(hh + 1) * 128], lhsT=khTM[:, c, hp * 128:(hp + 1) * 128],
                                                                                 rhs=gv[:, c, h * 128:(h + 1) * 128], start=True, stop=True), r=[khTM, gv], pw=[PS[7]])
                        fw.op("dve", lambda e, c=c, hp=hp: e.scalar_tensor_tensor(out=S[lo, hp, :], in0=S[lo, hp, :], scalar=ecl[lo, hp, c:c + 1], in1=PS[7][lo, 0:128],
                                                                                op0=ALU.mult, op1=ALU.add), r=[S, ecl, PS[7]], pw=[S])
                        fw.op("dve", lambda e, c=c, hp=hp: e.scalar_tensor_tensor(out=S[hi, hp, :], in0=S[hi, hp, :], scalar=ecl[hi, hp, c:c + 1], in1=PS[7][hi, 128:256],
                                                                                op0=ALU.mult, op1=ALU.add), r=[S, ecl, PS[7]], pw=[S])
                        fw.op("act", lambda e, hp=hp: e.activation(out=Sbf[:, hp, :], in_=S[:, hp, :], func=AF.Copy), r=[S], pw=[Sbf])
                        fw.op("dve", lambda e, hp=hp, cg=cg: e.tensor_scalar(out=memS[:, hp, :], in0=mem[:, hp, :], scalar1=decb[:, hp, cg:cg + 1], scalar2=None, op0=ALU.mult),
                              r=[mem, decb], pw=[memS])
                        fw.op("act", lambda e, hp=hp: e.activation(out=memSbf[:, hp, :], in_=memS[:, hp, :], func=AF.Copy), r=[memS], pw=[memSbf])
                        fw.op("dve", lambda e, hp=hp, cg=cg: e.tensor_scalar(out=nrmS[:, hp:hp + 1], in0=nrm[:, hp:hp + 1], scalar1=decb[:, hp, cg:cg + 1], scalar2=None, op0=ALU.mult),
                              r=[nrm, decb], pw=[nrmS])
                        fw.op("dve", lambda e, hp=hp: e.tensor_copy(out=nbc[:, hp, :], in_=nrmS[:, hp:hp + 1].to_broadcast([128, 128])), r=[nrmS], pw=[nbc])
                        for hh in range(2):
                            h = hp * 2 + hh
                            chunk_attention(c, h, ktl, mqs, mv, memSbf, ml_o[hh], den=(ml_d[hh], nbc))
                            fw.op("pe", lambda e, c=c, h=h, hh=hh, hp=hp: e.matmul(PS[7][:, hh * 128:(hh + 1) * 128], lhsT=ktlTM[:, c, hp * 128:(hp + 1) * 128],
                                                                                 rhs=mv[:, c, h * 128:(h + 1) * 128], start=True, stop=True), r=[ktlTM, mv], pw=[PS[7]])
                        fw.op("dve", lambda e, hp=hp: e.tensor_tensor(out=mem[lo, hp, :], in0=memS[lo, hp, :], in1=PS[7][lo, 0:128], op=ALU.add), r=[memS, PS[7]], pw=[mem])
                        fw.op("dve", lambda e, hp=hp: e.tensor_tensor(out=mem[hi, hp, :], in0=memS[hi, hp, :], in1=PS[7][hi, 128:256], op=ALU.add), r=[memS, PS[7]], pw=[mem])
                        fw.op("dve", lambda e, hp=hp, c=c: e.tensor_tensor(out=nrm[:, hp:hp + 1], in0=nrmS[:, hp:hp + 1], in1=ksum[:, hp, c:c + 1], op=ALU.add),
                              r=[nrmS, ksum], pw=[nrm])
                    for hh in range(2):
                        h = hp * 2 + hh
                        op_ = gla_o[hh]
                        osq, rs, tmp = osqs[hh], rss[hh], tmps[hh]
                        fw.op("act", lambda e, op_=op_: e.activation(out=osq[:], in_=op_[:], func=AF.Square), r=[op_], w=[osq])
                        fw.op("pe", lambda e: e.matmul(PS[6][:], lhsT=self.ones_bf[:], rhs=osq[:], start=True, stop=True), r=[self.ones_bf, osq], w=[PS[6]])
                        fw.op("act", lambda e: e.activation(out=rs[:], in_=PS[6][:], func=AF.Ln, scale=1.0 / 128.0, bias=self.eps_t[:, 0:1]), r=[PS[6], self.eps_t], w=[rs])
                        fw.op("act", lambda e: e.activation(out=rs[:], in_=rs[:], func=AF.Exp, scale=-0.5), r=[rs], w=[rs])
                        fw.op("dve", lambda e, op_=op_, h=h: e.scalar_tensor_tensor(out=tmp[:], in0=op_[:], scalar=gnw[:, h:h + 1], in1=rs[:], op0=ALU.mult, op1=ALU.mult),
                              r=[op_, gnw, rs], w=[tmp])
                        fw.op("dve", lambda e, h=h: e.tensor_tensor(out=ygla[:, h, :], in0=tmp[:], in1=gr[:, h, :], op=ALU.mult), r=[tmp, gr], pw=[ygla])
                    for hh in range(2):
                        h = hp * 2 + hh
                        op_, dp_ = ml_o[hh], ml_d[hh]
                        fw.op("pe", lambda e, h=h: e.matmul(PS[6][:], lhsT=self.selv[h][:], rhs=flo[:], start=True, stop=True), r=[self.selv[h], flo], w=[PS[6]])
                        fw.op("act", lambda e: e.activation(out=fls[:], in_=PS[6][:], func=AF.Copy), r=[PS[6]], w=[fls])
                        fw.op("dve", lambda e, dp_=dp_: e.tensor_tensor(out=tmp2[:], in0=dp_[:], in1=fls[:], op=ALU.max), r=[dp_, fls], w=[tmp2])
                        fw.op("dve", lambda e, dp_=dp_: e.scalar_tensor_tensor(out=tmp2[:], in0=dp_[:], scalar=-1.0, in1=tmp2[:], op0=ALU.mult, op1=ALU.max), r=[dp_, tmp2], w=[tmp2])
                        fw.op("act", lambda e: e.activation(out=tmp2[:], in_=tmp2[:], func=AF.Ln), r=[tmp2], w=[tmp2])
                        fw.op("act", lambda e: e.activation(out=tmp2[:], in_=tmp2[:], func=AF.Exp, scale=-1.0), r=[tmp2], w=[tmp2])
                        fw.op("dve", lambda e, op_=op_: e.tensor_tensor(out=tmp2[:], in0=op_[:], in1=tmp2[:], op=ALU.mult), r=[op_, tmp2], w=[tmp2])
                        fw.op("dve", lambda e, h=h: e.tensor_tensor(out=yml[:, h, :], in0=tmp2[:], in1=mo[:, h, :], op=ALU.mult), r=[tmp2, mo], pw=[yml])
                if st + 1 < self.NST:
                    issue_chain_loads(st + 1)
                if BSTOP <= 5:
                    continue
                if self.debug:
                    fw.dma("pool", self.s_ygla[:, t0:t0 + ST].rearrange("(a p) t -> p a t", p=128), ygla[:], r=[ygla], pw=[self.s_ygla])
                    fw.dma("pool", self.s_yml[:, t0:t0 + ST].rearrange("(a p) t -> p a t", p=128), yml[:], r=[yml], pw=[self.s_yml])
                k = 0
                for j in range(8):
                    gates = gts[j % 2]
                    mrg = mrgs[j % 2]
                    mtmp = mtmps[j % 2]
                    fw.dma("sp", gates[:], self.s_gates[:, t0:t0 + ST].rearrange("(b j p) t -> j p b t", b=3, p=128)[j], r=[self.s_gates], w=[gates])
                    for bi, yT in enumerate((ygla, yml, ys5)):
                        ps = PS[k % 4]
                        k += 1
                        for kc in range(4):
                            fw.op("pe", lambda e, kc=kc, j=j, bi=bi, yT=yT, ps=ps: e.matmul(ps[:], lhsT=pj[bi][:, kc, j * 128:(j + 1) * 128], rhs=yT[:, kc, :],
                                                                                          start=(kc == 0), stop=(kc == 3)), r=[pj[bi], yT], pw=[ps])
                        if bi == 0:
                            fw.op("dve", lambda e, j=j, ps=ps, gates=gates: e.tensor_tensor(out=mrg[:], in0=gates[:, 0, :], in1=ps[:], op=ALU.mult), r=[gates, ps], w=[mrg])
                        else:
                            mt = mtmp[bi - 1]
                            fw.op("dve", lambda e, j=j, ps=ps, bi=bi, mt=mt, gates=gates: e.tensor_tensor(out=mt[:], in0=gates[:, bi, :], in1=ps[:], op=ALU.mult), r=[gates, ps], w=[mt])
                            if bi == 1:
                                fw.op("pool", lambda e, mt=mt: e.tensor_tensor(out=mrg[:], in0=mrg[:], in1=mt[:], op=ALU.add), r=[mrg, mt], w=[mrg])
                            else:
                                fw.op("pool", lambda e, mt=mt, j=j: e.tensor_tensor(out=mT[:, j, :], in0=mrg[:], in1=mt[:], op=ALU.add), r=[mrg, mt], pw=[mT])
                for j in range(8):
                    ps = PS[4 + j % 2]
                    for kc in range(8):
                        fw.op("pe", lambda e, kc=kc, j=j, ps=ps: e.matmul(ps[:], lhsT=wo[:, kc, j * 128:(j + 1) * 128], rhs=mT[:, kc, :], start=(kc == 0), stop=(kc == 7)),
                              r=[wo, mT], pw=[ps])
                    fw.op("act", lambda e, j=j, ps=ps: e.activation(out=ysb[:, j, :], in_=ps[:], func=AF.Copy), r=[ps], pw=[ysb])
                    fw.op("act", lambda e, j=j, ps=ps: e.activation(out=sq[:, j, :], in_=ps[:], func=AF.Square), r=[ps], pw=[sq])
                self.postnorm(xs, ysb, sq, rstd, self.GT1, PS[7], ST)
                fw.dma("pool", self.xT[:, t0:t0 + ST].rearrange("(kc p) t -> p kc t", p=128), xs[:], r=[xs], pw=[self.xT])
                if self.debug:
                    fw.dma("pool", self.s_x1[:, t0:t0 + ST].rearrange("(kc p) t -> p kc t", p=128), xs[:], r=[xs], pw=[self.s_x1])
            fw.barrier()

    def phase_F(self, l):
        nc, fw, sb = self.nc, self.fw, self.sb
        PS = self.PS
        T = self.T
        FT = 256
        NJ = FH // 128
        NT = T // FT
        with ExitStack() as pes:
            w1 = sb(pes, "w1", [128, 8, 2 * FH], BF16)
            w2 = sb(pes, "w2", [128, NJ, D], BF16)
            NG = 2
            HJ = NJ // 2
            g_views = [TT(w1.t, "w1g%d" % i) for i in range(NG)]
            u_views = [TT(w1.t, "w1u%d" % i) for i in range(NG)]
            for gi in range(NG):
                a, b_ = gi * HJ * 128, (gi + 1) * HJ * 128
                for (base, v) in ((0, g_views[gi]), (FH, u_views[gi])):
                    for kc in range(8):
                        fw.dma("pool", w1[:, kc, base + a:base + b_], self.ffn_w1[l, kc * 128:(kc + 1) * 128, base + a:base + b_], pw=[v])
            for jc in range(NJ):
                fw.dma("pool", w2[:, jc, :], self.ffn_w2[l, jc * 128:(jc + 1) * 128, :], pw=[w2])
            xss = [sb(pes, "xsf%d" % i, [128, 8, FT], F32) for i in range(2)]
            hTs = [sb(pes, "hTf%d" % i, [128, 8, FT], BF16) for i in range(2)]
            sqx = sb(pes, "sqx", [128, 8, FT], BF16)
            sqy = sb(pes, "sqy", [128, 8, FT], BF16)
            rstdx = sb(pes, "rstdx", [128, FT], F32)
            rstdy = sb(pes, "rstdy", [128, FT], F32)
            aT = sb(pes, "aT", [128, NJ, FT], BF16)
            ysb = sb(pes, "ysbf", [128, 8, FT], F32)
            sil = [sb(pes, "sil%d" % i, [128, FT], F32) for i in range(2)]
            steps = []
            if l + 1 < self.L:
                wt = [sb(pes, "adawF%d" % i, [128, 8, 128], F32) for i in range(2)]
                steps = self.prep_steps(l + 1, wt, PS[7])
            per_tile = (len(steps) + NT - 1) // NT if steps else 0

            def do_prenorm(ti_):
                xs_ = xss[ti_ % 2]
                fw.dma("sp", xs_[:], self.xT[:, ti_ * FT:(ti_ + 1) * FT].rearrange("(kc p) t -> p kc t", p=128), r=[self.xT], w=[xs_])
                self.prenorm(xs_, hTs[ti_ % 2], sqx, rstdx, self.G2, self.SH2, PS[6], W=FT)

            do_prenorm(0)
            k = 0
            for ti in range(NT):
                t0 = ti * FT
                xs, hT = xss[ti % 2], hTs[ti % 2]
                for _ in range(per_tile):
                    if steps:
                        steps.pop(0)()
                for jc in range(NJ):
                    if jc == 8 and ti + 1 < NT:
                        do_prenorm(ti + 1)
                    psg = PS[(2 * k) % 4]
                    psu = PS[(2 * k + 1) % 4]
                    sl = sil[k % 2]
                    k += 1
                    gv_, uv_ = g_views[jc // HJ], u_views[jc // HJ]
                    for kc in range(8):
                        fw.op("pe", lambda e, kc=kc, jc=jc, psg=psg: e.matmul(psg[:, 0:FT], lhsT=w1[:, kc, jc * 128:(jc + 1) * 128], rhs=hT[:, kc, :], start=(kc == 0), stop=(kc == 7)),
                              r=[gv_, hT], pw=[psg])
                    for kc in range(8):
                        fw.op("pe", lambda e, kc=kc, jc=jc, psu=psu: e.matmul(psu[:, 0:FT], lhsT=w1[:, kc, FH + jc * 128:FH + (jc + 1) * 128], rhs=hT[:, kc, :], start=(kc == 0), stop=(kc == 7)),
                              r=[uv_, hT], pw=[psu])
                    fw.op("act", lambda e, sl=sl, psg=psg: e.activation(out=sl[:], in_=psg[:, 0:FT], func=AF.Silu), r=[psg], w=[sl])
                    fw.op("dve", lambda e, sl=sl, psu=psu, jc=jc: e.tensor_tensor(out=aT[:, jc, :], in0=sl[:], in1=psu[:, 0:FT], op=ALU.mult), r=[sl, psu], pw=[aT])
                for j in range(8):
                    ps = PS[4 + j % 2]
                    for jc in range(NJ):
                        fw.op("pe", lambda e, j=j, jc=jc, ps=ps: e.matmul(ps[:, 0:FT], lhsT=w2[:, jc, j * 128:(j + 1) * 128], rhs=aT[:, jc, :], start=(jc == 0), stop=(jc == NJ - 1)),
                              r=[w2, aT], pw=[ps])
                    fw.op("act", lambda e, j=j, ps=ps: e.activation(out=ysb[:, j, :], in_=ps[:, 0:FT], func=AF.Copy), r=[ps], pw=[ysb])
                    fw.op("act", lambda e, j=j, ps=ps: e.activation(out=sqy[:, j, :], in_=ps[:, 0:FT], func=AF.Square), r=[ps], pw=[sqy])
                self.postnorm(xs, ysb, sqy, rstdy, self.GT2, PS[6], FT)
                fw.dma("pool", self.xT[:, t0:t0 + FT].rearrange("(kc p) t -> p kc t", p=128), xs[:], r=[xs], pw=[self.xT])
            while steps:
                steps.pop(0)()
            fw.barrier()


def _col(v, n):
    return np.ascontiguousarray(v.reshape(n, 128).T)


def prep_inputs(inputs, b, T, L):
    f = lambda a: np.ascontiguousarray(np.asarray(a, dtype=np.float32))
    m = {}
    m["xT"] = f(np.asarray(inputs["x"])[b, :T, :].T)
    m["cT"] = _col(f(inputs["c"])[b], 8)
    m["ada_w"] = f(inputs["ada_w"])[:L]
    m["ada_bT"] = np.stack([_col(f(inputs["ada_b"])[l], 48) for l in range(L)])
    m["vecs"] = np.stack([np.stack([_col(f(inputs[k])[l], 8) for k in ("pre1_w", "post1_w", "pre2_w", "post2_w")], axis=1) for l in range(L)])
    m["w_in"] = f(inputs["w_in"])[:L]
    m["gla_a2"] = f(inputs["gla_a2"])[:L]
    m["gla_ab"] = np.stack([_col(f(inputs["gla_a_b"])[l], 2) for l in range(L)])
    m["gla_nw"] = np.stack([_col(f(inputs["gla_norm_w"])[l], 4) for l in range(L)])
    cw = f(inputs["ml_conv_w"])[:L]
    m["conv_w"] = np.ascontiguousarray(cw.reshape(L, 4, 4, 128).transpose(0, 3, 2, 1))
    m["conv_b"] = np.stack([_col(f(inputs["ml_conv_b"])[l], 4) for l in range(L)])
    m["ml_ifb"] = np.ascontiguousarray(np.stack([f(inputs["ml_i_b"])[:L], f(inputs["ml_f_b"])[:L]], axis=-1))
    dup = lambda a: np.ascontiguousarray(np.concatenate([a, a], axis=1))
    m["s5_lre"] = dup(f(inputs["s5_lam_re"])[:L].transpose(0, 2, 1))
    m["s5_lim"] = dup(f(inputs["s5_lam_im"])[:L].transpose(0, 2, 1))
    m["s5_ls"] = np.ascontiguousarray(np.broadcast_to(f(inputs["s5_log_step"])[:L, None, :], (L, 128, 32)))
    m["s5_bre"] = dup(f(inputs["s5_b_re"])[:L].transpose(0, 2, 1, 3))
    m["s5_bim"] = dup(f(inputs["s5_b_im"])[:L].transpose(0, 2, 1, 3))
    m["s5_cre"] = dup(f(inputs["s5_c_re"])[:L].transpose(0, 3, 1, 2))
    m["s5_cim"] = dup(f(inputs["s5_c_im"])[:L].transpose(0, 3, 1, 2))
    d = f(inputs["s5_d"])[:L].reshape(L, 32, 16)
    m["s5_dblk"] = np.ascontiguousarray(np.broadcast_to(d.transpose(0, 2, 1)[:, None, :, :], (L, 8, 16, 32)).reshape(L, 128, 32))
    m["glu_w"] = f(inputs["s5_glu_w"])[:L]
    m["glu_b"] = np.stack([_col(f(inputs["s5_glu_b"])[l], 4) for l in range(L)])
    m["proj_gla"] = f(inputs["proj_gla"])[:L]
    m["proj_ml"] = f(inputs["proj_ml"])[:L]
    m["proj_s5"] = f(inputs["proj_s5"])[:L]
    m["bgb"] = np.stack([_col(f(inputs["branch_gate_b"])[l], 24) for l in range(L)])
    m["w_out"] = f(inputs["w_out"])[:L]
    m["ffn_w1"] = f(inputs["ffn_w_in"])[:L]
    m["ffn_w2"] = f(inputs["ffn_w_out"])[:L]
    return m


_CACHE = {}


def run(inputs, T=4096, L=2, ncores=8, debug=False):
    key = (T, L, debug)
    if key not in _CACHE:
        _CACHE[key] = Prog(T, L, debug).build()
    nc = _CACHE[key]
    shared = None
    in_maps = []
    for b in range(ncores):
        m = prep_inputs(inputs, b, T, L)
        if shared is None:
            shared = m
        else:
            for k in m:
                if k not in ("xT", "cT"):
                    m[k] = shared[k]
        in_maps.append(m)
    res = run_bass_kernel_spmd(nc, in_maps, core_ids=list(range(ncores)))
    return res.results


def kernel(**inputs):
    results = run(inputs)
    out = np.stack([np.ascontiguousarray(r["outT"].T) for r in results], axis=0)
    return out.astype(np.float32)
```
